# Optimizing a Trainium2 kernel written in Bass

```python
import jax, jax.numpy as jnp
from jax import lax
import numpy as np

D_MODEL = 2048
BATCH = 8
SEQ = 2048
DEPTH = 4

HEAD_DIM = 64
N_ATT_HEADS = 12
N_RWKV_HEADS = 12
D_ATT = N_ATT_HEADS * HEAD_DIM
D_RWKV = N_RWKV_HEADS * HEAD_DIM
D_POOL = D_MODEL - D_ATT - D_RWKV
N_POOL_GROUPS = 4
POOL_GROUP = D_POOL // N_POOL_GROUPS
POOL_WINDOWS = (2, 4, 8, 16)
DILATED_BRANCHES = ((128, 1), (512, 4), (2048, 16))
ATT_BLOCK = 128
ROPE_THETA = 10000.0
RWKV_DECAY_LORA = 96
RWKV_AAA_LORA = 96
RWKV_GATE_LORA = 256
D_RWKV_IN = 3 * D_RWKV + RWKV_DECAY_LORA + RWKV_AAA_LORA + RWKV_GATE_LORA
D_IN = 3 * D_ATT + D_RWKV_IN + D_POOL
D_FF = 256 * ((8 * D_MODEL // 3 + 255) // 256)
NORM_EPS = 1e-6
RWKV_GN_EPS = 64e-5

kernel_name = 'hybrid_dilated_rwkv7_pool_macaron'


def rms_norm(x, g):
    xf = x.astype(jnp.float32)
    y = xf * lax.rsqrt(jnp.mean(xf * xf, axis=-1, keepdims=True) + NORM_EPS)
    return (y * g.astype(jnp.float32)).astype(x.dtype)


def swiglu(h, w_gate, w_up, w_down):
    return (jax.nn.silu(h @ w_gate) * (h @ w_up)) @ w_down


def rope_tables(seq_len, dtype):
    inv_freq = ROPE_THETA ** (-jnp.arange(0, HEAD_DIM, 2, dtype=jnp.float32) / HEAD_DIM)
    ang = jnp.arange(seq_len, dtype=jnp.float32)[:, None] * inv_freq[None, :]
    return jnp.cos(ang).astype(dtype), jnp.sin(ang).astype(dtype)


def rotary(x, cos, sin):
    x1, x2 = jnp.split(x, 2, axis=-1)
    c = cos[None, :, None, :]
    s = sin[None, :, None, :]
    return jnp.concatenate([x1 * c - x2 * s, x2 * c + x1 * s], axis=-1)


def to_strided(x, d):
    b, s = x.shape[:2]
    rest = x.shape[2:]
    x = jnp.moveaxis(x.reshape((b, s // d, d) + rest), 2, 1)
    return x.reshape((b * d, s // d) + rest)


def from_strided(x, b, d):
    l = x.shape[1]
    rest = x.shape[2:]
    x = jnp.moveaxis(x.reshape((b, d, l) + rest), 1, 2)
    return x.reshape((b, l * d) + rest)


def dilated_branch(q, k, v, window, dilation):
    b, s, h, dh = q.shape
    n_back = window // dilation
    qs, ks, vs = (to_strided(t, dilation) for t in (q, k, v))
    n, l = qs.shape[:2]
    nblk = -(-l // ATT_BLOCK)
    lp = nblk * ATT_BLOCK
    pad = ((0, 0), (0, lp - l), (0, 0), (0, 0))
    qs, ks, vs = (jnp.pad(t, pad).reshape(n, nblk, ATT_BLOCK, h, dh) for t in (qs, ks, vs))

    def with_prev(t):
        prev = jnp.pad(t[:, :-1], ((0, 0), (1, 0), (0, 0), (0, 0), (0, 0)))
        return jnp.concatenate([prev, t], axis=2)

    kb, vb = with_prev(ks), with_prev(vs)
    scores = jnp.einsum('nbqhd,nbkhd->nbhqk', qs, kb).astype(jnp.float32) * (dh ** -0.5)
    qi = jnp.arange(ATT_BLOCK)[:, None]
    kj = jnp.arange(2 * ATT_BLOCK)[None, :]
    dist = ATT_BLOCK + qi - kj
    key_pos = jnp.arange(nblk)[:, None, None] * ATT_BLOCK + kj - ATT_BLOCK
    valid = (dist >= 0) & (dist <= n_back) & (key_pos >= 0)
    scores = jnp.where(valid[None, :, None], scores, -jnp.inf)
    m = jnp.max(scores, axis=-1, keepdims=True)
    p = jnp.exp(scores - m)
    den = jnp.sum(p, axis=-1, keepdims=True)
    o = jnp.einsum('nbhqk,nbkhd->nbqhd', (p / den).astype(v.dtype), vb)
    lse = jnp.moveaxis((m + jnp.log(den))[..., 0], 2, 3)
    o = o.reshape(n, lp, h, dh)[:, :l]
    lse = lse.reshape(n, lp, h)[:, :l]
    return from_strided(o, b, dilation), from_strided(lse, b, dilation)


def dilated_attention(q, k, v):
    outs, lses = [], []
    for window, dilation in DILATED_BRANCHES:
        o, lse = dilated_branch(q, k, v, window, dilation)
        outs.append(o)
        lses.append(lse)
    alpha = jax.nn.softmax(jnp.stack(lses, axis=0), axis=0)
    out = jnp.einsum('gbsh,gbshd->bshd', alpha, jnp.stack(outs, axis=0).astype(jnp.float32))
    return out.astype(q.dtype)


def token_shift(x):
    return jnp.pad(x[:, :-1], ((0, 0), (1, 0), (0, 0)))


def rwkv7_mixer(z, mu, w_up, w0, a_up, a0, g_up, k_k, k_a, r_k, gn_w, gn_b):
    out_dtype = z.dtype
    z = z.astype(jnp.float32)
    b, s, _ = z.shape
    z = z + (token_shift(z) - z) * mu
    splits = [D_RWKV, 2 * D_RWKV, 3 * D_RWKV, 3 * D_RWKV + RWKV_DECAY_LORA,
              3 * D_RWKV + RWKV_DECAY_LORA + RWKV_AAA_LORA]
    r, k, v, zw, za, zg = jnp.split(z, splits, axis=-1)
    log_w = -jax.nn.softplus(-(w0 + jnp.tanh(zw) @ w_up)) - 0.5
    decay = jnp.exp(-jnp.exp(log_w))
    a = jax.nn.sigmoid(a0 + za @ a_up)
    g = jax.nn.sigmoid(zg) @ g_up
    kk = k * k_k
    k = k * (1.0 + (a - 1.0) * k_a)

    def heads(t):
        return t.reshape(b, s, N_RWKV_HEADS, HEAD_DIM).astype(jnp.float32)

    r, k, v, kk, a, decay = (heads(t) for t in (r, k, v, kk, a, decay))
    kk = kk / jnp.maximum(jnp.sqrt(jnp.sum(kk * kk, axis=-1, keepdims=True)), 1e-12)

    def step(state, inp):
        r_t, w_t, k_t, v_t, kk_t, a_t = inp
        sa = jnp.einsum('bhvk,bhk->bhv', state, kk_t)
        state = (state * w_t[:, :, None, :]
                 - sa[..., None] * (kk_t * a_t)[:, :, None, :]
                 + v_t[..., None] * k_t[:, :, None, :])
        return state, jnp.einsum('bhvk,bhk->bhv', state, r_t)

    state0 = jnp.zeros((b, N_RWKV_HEADS, HEAD_DIM, HEAD_DIM), jnp.float32)
    xs = tuple(jnp.moveaxis(t, 1, 0) for t in (r, decay, k, v, kk, a))
    _, y = lax.scan(step, state0, xs)
    y = jnp.moveaxis(y, 0, 1)
    mean = jnp.mean(y, axis=-1, keepdims=True)
    var = jnp.mean(jnp.square(y - mean), axis=-1, keepdims=True)
    y = ((y - mean) * lax.rsqrt(var + RWKV_GN_EPS)).reshape(b, s, D_RWKV) * gn_w + gn_b
    bonus = jnp.sum(r * k * r_k, axis=-1, keepdims=True) * v
    y = (y + bonus.reshape(b, s, D_RWKV)) * g
    return y.astype(out_dtype)


def pool_mixer(u, pool_w, pool_scale):
    b, s, _ = u.shape
    uf = u.astype(jnp.float32).reshape(b, s, N_POOL_GROUPS, POOL_GROUP)
    c = jnp.cumsum(uf, axis=1)
    t = jnp.arange(1, s + 1, dtype=jnp.float32)
    outs = []
    for gi, w in enumerate(POOL_WINDOWS):
        cg = c[:, :, gi]
        prev = jnp.pad(cg[:, :-w], ((0, 0), (w, 0), (0, 0)))
        mean = (cg - prev) / jnp.minimum(t, float(w))[None, :, None]
        outs.append(mean - uf[:, :, gi])
    p = jnp.stack(outs, axis=2).astype(u.dtype)
    y = jnp.einsum('bsgc,gcd->bsgd', p, pool_w).reshape(b, s, D_POOL)
    return y * pool_scale


def setup_inputs(seed: int = 0) -> dict:
    key = jax.random.key(seed)
    ks = iter(jax.random.split(key, 32))
    f32 = jnp.float32

    def nrm(shape, scale):
        return jax.random.normal(next(ks), shape, f32) * scale

    def gain(shape):
        return 1.0 + 0.05 * jax.random.normal(next(ks), shape, f32)

    def unif(shape, lo, hi):
        return jax.random.uniform(next(ks), shape, f32, lo, hi)

    L, D, F = DEPTH, D_MODEL, D_FF
    return {
        'x': nrm((BATCH, SEQ, D), 1.0),
        'ffn1_norm_pre': gain((L, D)),
        'ffn1_norm_post': gain((L, D)),
        'ffn1_w_gate': nrm((L, D, F), D ** -0.5),
        'ffn1_w_up': nrm((L, D, F), D ** -0.5),
        'ffn1_w_down': nrm((L, F, D), F ** -0.5),
        'mix_norm_pre': gain((L, D)),
        'mix_norm_post': gain((L, D)),
        'w_in': nrm((L, D, D_IN), D ** -0.5),
        'w_out': nrm((L, D, D), D ** -0.5),
        'rwkv_mu': unif((L, D_RWKV_IN), 0.0, 1.0),
        'rwkv_w_up': nrm((L, RWKV_DECAY_LORA, D_RWKV), 0.5 * RWKV_DECAY_LORA ** -0.5),
        'rwkv_w0': unif((L, D_RWKV), -6.0, -1.0),
        'rwkv_a_up': nrm((L, RWKV_AAA_LORA, D_RWKV), RWKV_AAA_LORA ** -0.5),
        'rwkv_a0': nrm((L, D_RWKV), 0.1),
        'rwkv_g_up': nrm((L, RWKV_GATE_LORA, D_RWKV), RWKV_GATE_LORA ** -0.5),
        'rwkv_k_k': 0.85 + nrm((L, D_RWKV), 0.05),
        'rwkv_k_a': gain((L, D_RWKV)),
        'rwkv_r_k': nrm((L, N_RWKV_HEADS, HEAD_DIM), 0.1),
        'rwkv_gn_w': gain((L, D_RWKV)),
        'rwkv_gn_b': nrm((L, D_RWKV), 0.01),
        'pool_w': nrm((L, N_POOL_GROUPS, POOL_GROUP, POOL_GROUP), POOL_GROUP ** -0.5),
        'pool_scale': gain((L, D_POOL)),
        'ffn2_norm_pre': gain((L, D)),
        'ffn2_norm_post': gain((L, D)),
        'ffn2_w_gate': nrm((L, D, F), D ** -0.5),
        'ffn2_w_up': nrm((L, D, F), D ** -0.5),
        'ffn2_w_down': nrm((L, F, D), F ** -0.5),
    }


def reference(x, ffn1_norm_pre, ffn1_norm_post, ffn1_w_gate, ffn1_w_up, ffn1_w_down,
              mix_norm_pre, mix_norm_post, w_in, w_out, rwkv_mu, rwkv_w_up, rwkv_w0,
              rwkv_a_up, rwkv_a0, rwkv_g_up, rwkv_k_k, rwkv_k_a, rwkv_r_k, rwkv_gn_w,
              rwkv_gn_b, pool_w, pool_scale, ffn2_norm_pre, ffn2_norm_post, ffn2_w_gate,
              ffn2_w_up, ffn2_w_down):
    b, s, _ = x.shape
    cos, sin = rope_tables(s, x.dtype)
    for i in range(DEPTH):
        h = rms_norm(x, ffn1_norm_pre[i])
        x = x + 0.5 * rms_norm(swiglu(h, ffn1_w_gate[i], ffn1_w_up[i], ffn1_w_down[i]),
                               ffn1_norm_post[i])
        h = rms_norm(x, mix_norm_pre[i])
        z = h @ w_in[i]
        zq, zk, zv, zr, zp = jnp.split(
            z, [D_ATT, 2 * D_ATT, 3 * D_ATT, 3 * D_ATT + D_RWKV_IN], axis=-1)
        q = rotary(zq.reshape(b, s, N_ATT_HEADS, HEAD_DIM), cos, sin)
        k = rotary(zk.reshape(b, s, N_ATT_HEADS, HEAD_DIM), cos, sin)
        v = zv.reshape(b, s, N_ATT_HEADS, HEAD_DIM)
        y_att = dilated_attention(q, k, v).reshape(b, s, D_ATT)
        y_rwkv = rwkv7_mixer(zr, rwkv_mu[i], rwkv_w_up[i], rwkv_w0[i], rwkv_a_up[i],
                             rwkv_a0[i], rwkv_g_up[i], rwkv_k_k[i], rwkv_k_a[i],
                             rwkv_r_k[i], rwkv_gn_w[i], rwkv_gn_b[i])
        y_pool = pool_mixer(zp, pool_w[i], pool_scale[i])
        mix = jnp.concatenate([y_att, y_rwkv, y_pool], axis=-1) @ w_out[i]
        x = x + rms_norm(mix, mix_norm_post[i])
        h = rms_norm(x, ffn2_norm_pre[i])
        x = x + 0.5 * rms_norm(swiglu(h, ffn2_w_gate[i], ffn2_w_up[i], ffn2_w_down[i]),
                               ffn2_norm_post[i])
    return x
```

```python
import numpy as np
from contextlib import ExitStack
import concourse.bass as bass
import concourse.mybir as mybir
from concourse.bass_utils import run_bass_kernel_spmd

F32 = mybir.dt.float32
BF16 = mybir.dt.bfloat16
AF = mybir.ActivationFunctionType
ALU = mybir.AluOpType

D = 2048
S = 2048
L = 4
DFF = 5632
NFC = DFF // 128
NDC = D // 128
TT = 512
NTT = S // TT
DATT = 768
DRW = 768
DPOOL = 512
DIN = 5568
RW_IN = 2752
EPS = 1e-6
GN_EPS = 64e-5


class Buf:
    def __init__(self, t, shape, name):
        self.t = t
        self.shape = tuple(shape)
        self.name = name

    def __getitem__(self, idx):
        if not isinstance(idx, tuple):
            idx = (idx,)
        box = []
        for d, n in enumerate(self.shape):
            if d < len(idx):
                i = idx[d]
                if isinstance(i, slice):
                    lo = 0 if i.start is None else i.start
                    hi = n if i.stop is None else i.stop
                    box.append((lo, hi))
                else:
                    box.append((i, i + 1))
            else:
                box.append((0, n))
        v = View(self.t[idx], self, tuple(box))
        if getattr(self, "psum", False):
            if getattr(self, "bank", None) is not None:
                v.banks = (self.bank,)
            else:
                i1 = idx[1] if len(idx) > 1 else slice(None)
                if isinstance(i1, slice):
                    v.banks = tuple(range(*i1.indices(self.shape[1])))
                else:
                    v.banks = (i1,)
        return v

    def all(self):
        return self[tuple(slice(None) for _ in self.shape)]


class View:
    banks = None

    def __init__(self, ap, buf, box):
        self.ap = ap
        self.buf = buf
        self.box = box

    def m(self, fn):
        v = View(fn(self.ap), self.buf, self.box)
        v.banks = self.banks
        return v


def _overlap(a, b):
    for (l0, h0), (l1, h1) in zip(a, b):
        if h0 <= l1 or h1 <= l0:
            return False
    return True


def _covers(a, b):
    for (l0, h0), (l1, h1) in zip(a, b):
        if l0 > l1 or h0 < h1:
            return False
    return True


class Op:
    __slots__ = ("eng", "fn", "deps", "dma", "waits", "signal", "pos", "sem", "val", "clock")

    def __init__(self, eng, fn, deps, dma):
        self.eng = eng
        self.fn = fn
        self.deps = deps
        self.dma = dma
        self.waits = []
        self.signal = False
        self.sem = None
        self.val = None


class Sched:
    ENGS = ("pe", "act", "dve", "pool", "sp")
    NDMA_SEM = 40
    EPOCH = 8000

    def __init__(self):
        self.ops = []
        self.track = {}
        self.final = []
        self.bar = None
        self.last = {}
        self.dmas = []
        self.psum_last = {}

    def add(self, eng, fn, reads=(), writes=(), dma=False):
        i = len(self.ops)
        deps = set()
        for v in list(reads) + list(writes):
            if v.banks is not None:
                for bk in v.banks:
                    lb = self.psum_last.setdefault(bk, {})
                    for f_, j_ in lb.items():
                        if f_ != eng:
                            deps.add(j_)
                    lb[eng] = i
        reads = [v for v in reads if v.banks is None]
        writes = [v for v in writes if v.banks is None]
        for v in reads:
            ents = self.track.setdefault(v.buf.name, [])
            for e in ents:
                if _overlap(e[0], v.box):
                    if e[1] is not None:
                        deps.add(e[1])
                    rd = e[2]
                    if dma:
                        if i not in rd:
                            rd.append(i)
                    else:
                        for k in range(len(rd)):
                            if rd[k] == i:
                                break
                            o = self.ops[rd[k]]
                            if (not o.dma) and o.eng == eng:
                                rd[k] = i
                                break
                        else:
                            rd.append(i)
        for v in writes:
            ents = self.track.setdefault(v.buf.name, [])
            keep = []
            for e in ents:
                if _overlap(e[0], v.box):
                    if e[1] is not None:
                        deps.add(e[1])
                    deps.update(e[2])
                    if _covers(v.box, e[0]):
                        continue
                keep.append(e)
            keep.append([v.box, i, []])
            self.track[v.buf.name] = keep
        deps.discard(i)
        if self.bar is not None:
            deps.add(self.bar)
        self.ops.append(Op(eng, fn, deps, dma))
        if dma:
            self.dmas.append(i)
        else:
            self.last[eng] = i
        return i

    def barrier(self, fn):
        i = len(self.ops)
        deps = set(self.last.values()) | set(self.dmas)
        self.ops.append(Op("dve", fn, deps, False))
        self.last["dve"] = i
        self.dmas = []
        self.bar = i
        return i

    def plan(self):
        clock = {e: {} for e in self.ENGS}
        pos = {e: 0 for e in self.ENGS}
        dma_rr = 0
        dma_cnt = [0] * self.NDMA_SEM
        dma_last = [None] * self.NDMA_SEM
        ops = self.ops
        for i, op in enumerate(ops):
            E = op.eng
            ck = clock[E]
            newck = None
            waits = []
            need = sorted(op.deps, reverse=True)
            if op.dma:
                s = dma_rr % self.NDMA_SEM
                dma_rr += 1
                dma_cnt[s] += 1
                op.sem = ("d", s)
                op.val = 16 * dma_cnt[s]
                if dma_last[s] is not None:
                    need.append(dma_last[s])
                dma_last[s] = i
            else:
                op.pos = pos[E]
                pos[E] += 1
            for j in need:
                oj = ops[j]
                if oj.dma:
                    key, val = oj.sem, oj.val
                else:
                    if oj.eng == "pe" and E == "pe" and not op.dma:
                        continue
                    key, val = ("c", oj.eng), oj.pos
                cur = ck if newck is None else newck
                if cur.get(key, -1) >= val:
                    continue
                if newck is None:
                    newck = dict(ck)
                for k2, v2 in oj.clock.items():
                    if newck.get(k2, -1) < v2:
                        newck[k2] = v2
                waits.append(j)
                oj.signal = True
            if newck is not None:
                clock[E] = newck
                ck = newck
            op.waits = waits
            c = dict(ck)
            if op.dma:
                c[op.sem] = op.val
            else:
                c[("c", E)] = op.pos
            op.clock = c
        for op in ops:
            op.clock = None

    def emit(self, nc, es):
        self.plan()
        ops = self.ops
        cnt = {e: 0 for e in self.ENGS}
        nep = {e: 1 for e in self.ENGS}
        for op in ops:
            if op.dma or not op.signal:
                continue
            E = op.eng
            cnt[E] += 1
            ep = (cnt[E] - 1) // self.EPOCH
            op.sem = ("c", E, ep)
            op.val = cnt[E] - ep * self.EPOCH
            nep[E] = max(nep[E], ep + 1)
        sems = {}
        for E in self.ENGS:
            for ep in range(nep[E]):
                sems[("c", E, ep)] = es.enter_context(nc.semaphore("s_%s_%d" % (E, ep)))
        for s in range(self.NDMA_SEM):
            sems[("d", s)] = es.enter_context(nc.semaphore("s_dma_%d" % s))
        per = {e: [] for e in self.ENGS}
        for i, op in enumerate(ops):
            per[op.eng].append(op)
        final = self.final
        block = es.enter_context(nc.Block())

        def run(E, eng):
            for op in per[E]:
                w = {}
                for j in op.waits:
                    oj = ops[j]
                    if w.get(oj.sem, -1) < oj.val:
                        w[oj.sem] = oj.val
                for k, v in w.items():
                    eng.wait_ge(sems[k], v)
                ins = op.fn(eng)
                if op.dma:
                    ins.then_inc(sems[op.sem], 16)
                elif op.signal:
                    ins.then_inc(sems[op.sem], 1)
            for (fe, j) in final:
                if fe == E:
                    oj = ops[j]
                    eng.wait_ge(sems[oj.sem], oj.val)

        @block.tensor
        def _(e):
            run("pe", e)

        @block.scalar
        def _(e):
            run("act", e)

        @block.vector
        def _(e):
            run("dve", e)

        @block.gpsimd
        def _(e):
            run("pool", e)

        @block.sync
        def _(e):
            run("sp", e)


U8 = mybir.dt.uint8
ISZ = {F32: 4, BF16: 2}
ARENA = 206 * 1024


class SBuf:
    def __init__(self, arena_t, off, shape, dt, name):
        self.name = name
        self.shape = tuple(shape)
        self.isz = ISZ[dt]
        n = 1
        for d in shape[1:]:
            n *= d
        self.nbytes = n * self.isz
        ap = arena_t[:, off:off + self.nbytes].bitcast(dt)
        if len(shape) == 3:
            ap = ap.rearrange("p (a b) -> p a b", a=shape[1])
        elif len(shape) == 4:
            ap = ap.rearrange("p (a b c) -> p a b c", a=shape[1], b=shape[2])
        if shape[0] < 128:
            ap = ap[0:shape[0]]
        self.t = ap

    __getitem__ = Buf.__getitem__
    all = Buf.all


class Prog:
    def __init__(self, nc, es, cfg):
        self.nc = nc
        self.es = es
        self.cfg = cfg
        self.s = Sched()
        self.arena = es.enter_context(nc.sbuf_tensor("arena", [128, ARENA], U8))
        self.top = 0
        self.nname = 0
        self.barsc = self.sb("barsc", [128, 8], F32)

    def sb(self, name, shape, dt):
        n = 1
        for d in shape[1:]:
            n *= d
        nb = (n * ISZ[dt] + 63) // 64 * 64
        assert self.top + nb <= ARENA, "SBUF arena overflow at %s: %d" % (name, self.top + nb)
        self.nname += 1
        b = SBuf(self.arena, self.top, shape, dt, "%s#%d" % (name, self.nname))
        self.top += nb
        return b

    def mark(self):
        return self.top

    def release(self, m):
        self.barrier()
        self.top = m

    def barrier(self):
        ap = self.barsc.all().ap
        self.s.barrier(lambda e: e.memset(ap, 0.0))

    def ps_all(self):
        t = self.es.enter_context(self.nc.psum_tensor("PS", [128, 8, 512], F32))
        full = Buf(t, (128, 8, 512), "PS")
        full.psum = True
        full.bank = None
        banks = []
        for i in range(8):
            bk = Buf(t[:, i, :], (128, 512), "pb%d" % i)
            bk.psum = True
            bk.bank = i
            banks.append(bk)
        return full, banks

    def dram(self, name, shape, dt, kind="Internal"):
        t = self.nc.dram_tensor(name, list(shape), dt, kind=kind)
        return Buf(t, shape, name)

    def mm(self, out, lhsT, rhs, start=True, stop=True, **kw):
        self.s.add("pe", lambda e: e.matmul(out.ap, lhsT.ap, rhs.ap, start=start, stop=stop, **kw),
                   reads=[lhsT, rhs], writes=[out])

    def transpose(self, out, in_, ident):
        self.s.add("pe", lambda e: e.transpose(out.ap, in_.ap, ident.ap),
                   reads=[in_, ident], writes=[out])

    def act(self, out, in_, func, scale=1.0, bias=None, accum=None):
        rd = [in_]
        kw = {}
        if isinstance(scale, View):
            rd.append(scale)
            kw["scale"] = scale.ap
        else:
            kw["scale"] = scale
        if isinstance(bias, View):
            rd.append(bias)
            kw["bias"] = bias.ap
        elif bias is not None:
            kw["bias"] = bias
        wr = [out]
        if accum is not None:
            kw["accum_out"] = accum.ap
            wr.append(accum)
        self.s.add("act", lambda e: e.activation(out=out.ap, in_=in_.ap, func=func, **kw),
                   reads=rd, writes=wr)

    def tt(self, out, a, b, op, eng="dve"):
        eng = "dve"
        self.s.add(eng, lambda e: e.tensor_tensor(out.ap, a.ap, b.ap, op), reads=[a, b], writes=[out])

    def ts(self, out, a, s1, s2=None, op0=ALU.mult, op1=None, eng="dve"):
        eng = "dve"
        rd = [a]
        v1 = s1
        v2 = s2
        if isinstance(s1, View):
            rd.append(s1)
            v1 = s1.ap
        if isinstance(s2, View):
            rd.append(s2)
            v2 = s2.ap
        if op1 is None:
            self.s.add(eng, lambda e: e.tensor_scalar(out.ap, a.ap, v1, None, op0), reads=rd, writes=[out])
        else:
            self.s.add(eng, lambda e: e.tensor_scalar(out.ap, a.ap, v1, v2, op0, op1), reads=rd, writes=[out])

    def stt(self, out, in0, scalar, in1, op0, op1):
        rd = [in0, in1]
        sv = scalar
        if isinstance(scalar, View):
            rd.append(scalar)
            sv = scalar.ap
        self.s.add("dve", lambda e: e.scalar_tensor_tensor(out.ap, in0.ap, sv, in1.ap, op0, op1),
                   reads=rd, writes=[out])

    def copy(self, out, in_, eng="dve"):
        eng = "dve"
        self.s.add(eng, lambda e: e.tensor_copy(out.ap, in_.ap), reads=[in_], writes=[out])

    def recip(self, out, in_):
        self.s.add("dve", lambda e: e.reciprocal(out.ap, in_.ap), reads=[in_], writes=[out])

    def memset(self, out, val, eng="dve"):
        eng = "dve"
        self.s.add(eng, lambda e: e.memset(out.ap, val), reads=[], writes=[out])

    def scan(self, out, d0, d1, init, op0, op1):
        self.s.add("dve", lambda e: e.tensor_tensor_scan(out.ap, d0.ap, d1.ap, init, op0, op1),
                   reads=[d0, d1], writes=[out])

    def bn_stats(self, out, in_):
        self.s.add("dve", lambda e: e.bn_stats(out.ap, in_.ap), reads=[in_], writes=[out])

    def bn_aggr(self, out, in_):
        self.s.add("dve", lambda e: e.bn_aggr(out.ap, in_.ap), reads=[in_], writes=[out])

    def dma(self, eng, out, in_):
        return self.s.add(eng, lambda e: e.dma_start(out=out.ap, in_=in_.ap), reads=[in_], writes=[out], dma=True)


def rr(gens):
    gens = list(gens)
    while gens:
        nxt = []
        for g in gens:
            try:
                next(g)
                nxt.append(g)
            except StopIteration:
                pass
        gens = nxt


NPV = 56
C05 = float(np.exp(-0.5))
ZCH = ([("q", i * 128, 128, i) for i in range(6)] + [("k", 768 + i * 128, 128, i) for i in range(6)]
       + [("rkv", 2304 + i * 128, 128, i) for i in range(18)]
       + [("zw", 4608, 96, 0), ("za", 4704, 96, 0), ("zg", 4800, 128, 0), ("zg", 4928, 128, 1)]
       + [("pool", 5056 + i * 128, 128, i) for i in range(4)])
ZS_RKV, ZS_ZW, ZS_ZA, ZS_ZG, ZS_POOL = 0, 18, 19, 20, 22
NZS = 26


def build(cfg):
    nc = bass.Bass("TRN2", target_bir_lowering=False)
    es = ExitStack()
    P = Prog(nc, es, cfg)
    n_layers = cfg.get("n_layers", L)
    dbg = cfg.get("dbg", False)

    def ext(name, shape):
        return P.dram(name, shape, F32, kind="ExternalInput")

    x_in = ext("x", [S, D])
    prm = {}
    for nm, shp in [
        ("ffn1_norm_pre", [L * NDC, 128]), ("ffn1_norm_post", [L * NDC, 128]),
        ("ffn1_w_gate", [L, D, DFF]), ("ffn1_w_up", [L, D, DFF]), ("ffn1_w_down", [L, DFF, D]),
        ("mix_norm_pre", [L * NDC, 128]), ("mix_norm_post", [L * NDC, 128]),
        ("w_in", [L, D, DIN]), ("w_out", [L, D, D]),
        ("ffn2_norm_pre", [L * NDC, 128]), ("ffn2_norm_post", [L * NDC, 128]),
        ("ffn2_w_gate", [L, D, DFF]), ("ffn2_w_up", [L, D, DFF]), ("ffn2_w_down", [L, DFF, D]),
        ("rwkv_w_up", [L, 96, 768]), ("rwkv_a_up", [L, 96, 768]), ("rwkv_g_up", [L, 256, 768]),
        ("pool_w", [L, 4, 128, 128]), ("pvec", [L * NPV, 128]),
        ("gnw", [L, 128, 384]), ("gnb", [L, 128, 384]),
        ("c_ident", [128, 128]), ("c_prot", [128, 128]), ("c_cos", [128, S]), ("c_sin", [128, S]),
        ("c_masks", [128, 4, 128]), ("c_e2", [128, 64]), ("c_scanmask", [128, 512]),
        ("c_poolfac", [128, 4, 16]), ("c_blockones", [128, 128]),
    ]:
        prm[nm] = ext(nm, shp)
    out_d = P.dram("out", [S, D], F32, kind="ExternalOutput")
    XT = P.dram("XT", [NDC, 128, S], F32)
    QK = P.dram("QK", [12, 128, S], BF16)
    VT = P.dram("VT", [S, DATT], BF16)
    ZS = P.dram("ZS", [NZS, 128, S], F32)
    YM = P.dram("YM", [NDC, 128, S], BF16, kind=("ExternalOutput" if dbg else "Internal"))

    ident = P.sb("identf", [128, 128], F32)
    identb = P.sb("identb", [128, 128], BF16)
    ones = P.sb("onesf", [128, 128], F32)
    onesb = P.sb("onesb", [128, 128], BF16)
    epsb = P.sb("epsb", [128, 1], F32)
    gneps = P.sb("gneps", [128, 1], F32)
    gains = {}
    for nm in ["ffn1_norm_pre", "ffn1_norm_post", "mix_norm_pre", "mix_norm_post",
               "ffn2_norm_pre", "ffn2_norm_post"]:
        gains[nm] = P.sb("g_" + nm, [128, L * NDC], F32)
    pvec = P.sb("pvec", [128, L * NPV], F32)
    omka = P.sb("omka", [128, L * 6], F32)
    NWA = 4
    NWD = 3
    NQF = NFC // 4
    wA = [P.sb("wA%d" % i, [128, NDC, 256], BF16) for i in range(NWA)]
    PSF, pb = P.ps_all()
    M0 = P.mark()

    xs = P.sb("xs", [128, NDC, TT], F32)
    hb = P.sb("hb", [128, NDC, TT], BF16)
    actb = P.sb("actb", [128, NFC, TT], BF16)
    yb = P.sb("yb", [128, NDC, TT], F32)
    sq = [P.sb("sq%d" % i, [128, TT], F32) for i in range(2)]
    sg = sq
    rstd = P.sb("rstd", [128, TT], F32)
    gstage = P.sb("gstage", [128, 128], F32)
    wD = [P.sb("wD%d" % i, [128, NQF, 512], BF16) for i in range(NWD)]
    FFN_TOP = P.mark()

    for bk in pb:
        P.memset(bk.all(), 0.0)
    P.dma("sp", ident.all(), prm["c_ident"].all())
    P.dma("pool", identb.all(), prm["c_ident"].all())
    P.memset(ones.all(), 1.0)
    P.memset(onesb.all(), 1.0)
    P.memset(epsb.all(), EPS)
    P.memset(gneps.all(), GN_EPS)
    for gi, nm in enumerate(gains):
        P.dma("sp", gstage[0:L * NDC, :], prm[nm].all())
        P.transpose(pb[7][:, 0:L * NDC], gstage[0:L * NDC, :], ident[0:L * NDC, 0:L * NDC])
        P.copy(gains[nm].all(), pb[7][:, 0:L * NDC])
    for l in range(L):
        P.dma("sp", gstage[0:NPV, :], prm["pvec"][l * NPV:(l + 1) * NPV, :])
        P.transpose(pb[7][:, 0:NPV], gstage[0:NPV, :], ident[0:NPV, 0:NPV])
        P.copy(pvec[:, l * NPV:(l + 1) * NPV], pb[7][:, 0:NPV])
        P.ts(omka[:, l * 6:(l + 1) * 6], pvec[:, l * NPV + 28:l * NPV + 34], -1.0, 1.0, ALU.mult, ALU.add)

    wq = []
    NSLOT = {"A": NWA, "D": NWD}
    wstate = {"issued": 0, "slots": {}, "rel": {"A": 0, "D": 0}, "cnt": {"A": 0, "D": 0}}

    def w_ensure(k):
        while wstate["issued"] < min(k + 1, len(wq)):
            i = wstate["issued"]
            cls, src, wdt = wq[i]
            if wstate["cnt"][cls] - wstate["rel"][cls] >= NSLOT[cls]:
                break
            slot = (wA if cls == "A" else wD)[wstate["cnt"][cls] % NSLOT[cls]]
            wstate["cnt"][cls] += 1
            wstate["slots"][i] = slot
            wstate.setdefault("dix", {})[i] = P.dma("pool", slot[:, :, 0:wdt], src)
            wstate["issued"] += 1

    def w_get(k, depth=4):
        w_ensure(k + depth)
        assert k in wstate["slots"], "weight %d not issued" % k
        return wstate["slots"][k]

    def w_release(k):
        wstate["rel"][wq[k][0]] += 1
        del wstate["slots"][k]

    def wsrc(buf, l, c0, wdt):
        return buf[l, :, c0:c0 + wdt].m(lambda a: a.rearrange("(c p) f -> p c f", p=128))

    def xt_tile(tq):
        return XT[:, :, tq * TT:(tq + 1) * TT].m(lambda a: a.rearrange("c p t -> p c t"))

    def input_transpose():
        for tb in range(S // 128):
            P.dma("sp", xs[:, 0:4, :],
                  x_in[tb * 128:(tb + 1) * 128, :].m(lambda a: a.rearrange("p (c f) -> p c f", c=4)))
            for g4 in range(4):
                bank = pb[(tb * 4 + g4) % 4]
                for j in range(4):
                    dc = g4 * 4 + j
                    P.transpose(bank[:, j * 128:(j + 1) * 128],
                                xs[:, dc // 4, (dc % 4) * 128:(dc % 4 + 1) * 128], ident.all())
                st = yb[:, (tb * 4 + g4) % NDC, :]
                if (tb * 4 + g4) % 2 == 0:
                    P.copy(st, bank.all())
                else:
                    P.act(st, bank.all(), AF.Copy)
                P.dma("sp", XT[g4 * 4:(g4 + 1) * 4, :, tb * 128:(tb + 1) * 128].m(
                    lambda a: a.rearrange("c p t -> p c t")),
                    st.m(lambda a: a.rearrange("p (c t) -> p c t", c=4)))

    def output_transpose():
        for tq in range(NTT):
            P.dma("sp", xs.all(), xt_tile(tq))
            for tb in range(TT // 128):
                for g4 in range(4):
                    bank = pb[(tb * 4 + g4) % 4]
                    for j in range(4):
                        dc = g4 * 4 + j
                        P.transpose(bank[:, j * 128:(j + 1) * 128],
                                    xs[:, dc, tb * 128:(tb + 1) * 128], ident.all())
                    st = yb[:, (tb * 4 + g4) % NDC, :]
                    if (tb * 4 + g4) % 2 == 0:
                        P.copy(st, bank.all())
                    else:
                        P.act(st, bank.all(), AF.Copy)
                    r0 = tq * TT + tb * 128
                    di = P.dma("sp", out_d[r0:r0 + 128, g4 * 512:(g4 + 1) * 512], st)
                    P.s.final.append(("sp", di))

    def rms_rstd(src_of_dc, stat_bank, rs, sqs):
        for dc in range(NDC):
            t = sqs[dc % 2]
            P.act(t.all(), src_of_dc(dc), AF.Square)
            P.mm(stat_bank.all(), ones.all(), t.all(), start=(dc == 0), stop=(dc == NDC - 1))
        P.act(rs.all(), stat_bank.all(), AF.Sqrt, scale=1.0 / D, bias=epsb.all())
        P.recip(rs.all(), rs.all())

    def ffn_enqueue(l, which):
        wg, wu, wd = prm["ffn%d_w_gate" % which], prm["ffn%d_w_up" % which], prm["ffn%d_w_down" % which]
        base = len(wq)
        for fcp in range(NFC // 2):
            wq.append(("A", wsrc(wg, l, fcp * 256, 256), 256))
            wq.append(("A", wsrc(wu, l, fcp * 256, 256), 256))
        for dcg in range(4):
            for q in range(4):
                src = wd[l, q * NQF * 128:(q + 1) * NQF * 128, dcg * 512:(dcg + 1) * 512].m(
                    lambda a: a.rearrange("(c p) f -> p c f", p=128))
                wq.append(("D", src, 512))
        return base

    def post_residual(l, gpost, scale, tq):
        rms_rstd(lambda dc: yb[:, dc, :], pb[6], rstd, sq)
        for dc in range(NDC):
            P.stt(yb[:, dc, :], yb[:, dc, :], gpost[:, l * NDC + dc:l * NDC + dc + 1], rstd.all(), ALU.mult, ALU.mult)
            P.stt(xs[:, dc, :], yb[:, dc, :], scale, xs[:, dc, :], ALU.mult, ALU.add)
        P.dma("sp", xt_tile(tq), xs.all())

    def ffn_tile(l, which, tq, base):
        gpre = gains["ffn%d_norm_pre" % which]
        gpost = gains["ffn%d_norm_post" % which]
        P.dma("sp", xs.all(), xt_tile(tq))
        rms_rstd(lambda dc: xs[:, dc, :], pb[6], rstd, sq)
        for dc in range(NDC):
            P.stt(hb[:, dc, :], xs[:, dc, :], gpre[:, l * NDC + dc:l * NDC + dc + 1], rstd.all(), ALU.mult, ALU.mult)
        for fcp in range(NFC // 2):
            wgs = w_get(base + 2 * fcp, depth=3)
            wus = w_get(base + 2 * fcp + 1, depth=2)
            for sub in range(2):
                fc = 2 * fcp + sub
                cs_ = slice(sub * 128, (sub + 1) * 128)
                gps = pb[fc % 2]
                ups = pb[2 + fc % 2]
                for dc in range(NDC):
                    P.mm(gps.all(), wgs[:, dc, cs_], hb[:, dc, :], start=(dc == 0), stop=(dc == NDC - 1))
                for dc in range(NDC):
                    P.mm(ups.all(), wus[:, dc, cs_], hb[:, dc, :], start=(dc == 0), stop=(dc == NDC - 1))
                s_ = sg[fc % 2]
                P.act(s_.all(), gps.all(), AF.Silu)
                P.tt(actb[:, fc, :], s_.all(), ups.all(), ALU.mult)
            w_release(base + 2 * fcp)
            w_release(base + 2 * fcp + 1)
        for dcg in range(4):
            banks = pb[0:4] if dcg % 2 == 0 else pb[4:8]
            for q in range(4):
                kq = base + NFC + dcg * 4 + q
                wds = w_get(kq, depth=2)
                for dj in range(4):
                    for f_ in range(NQF):
                        P.mm(banks[dj].all(), wds[:, f_, dj * 128:(dj + 1) * 128], actb[:, q * NQF + f_, :],
                             start=(q == 0 and f_ == 0), stop=(q == 3 and f_ == NQF - 1))
                w_release(kq)
            for dj in range(4):
                if dj % 2 == 0:
                    P.act(yb[:, dcg * 4 + dj, :], banks[dj].all(), AF.Copy)
                else:
                    P.copy(yb[:, dcg * 4 + dj, :], banks[dj].all())
        post_residual(l, gpost, 0.5, tq)

    def mix_enqueue(l):
        base = len(wq)
        for (kind, c0, wdt, idx) in ZCH:
            wq.append(("A", wsrc(prm["w_in"], l, c0, wdt), wdt))
        for tq in range(NTT):
            for dc in range(NDC):
                wq.append(("A", wsrc(prm["w_out"], l, dc * 128, 128), 128))
        return base

    def mixer(l, base):
        pv0 = l * NPV
        P.release(M0)
        hall = P.sb("hall", [128, NDC, S], BF16)
        mA = P.mark()
        mxs = P.sb("mxs", [128, NDC, TT], F32)
        msq = [P.sb("msq%d" % i, [128, TT], F32) for i in range(2)]
        mrs = P.sb("mrs", [128, TT], F32)
        gpre = gains["mix_norm_pre"]
        for tq in range(NTT):
            P.dma("sp", mxs.all(), xt_tile(tq))
            rms_rstd(lambda dc: mxs[:, dc, :], pb[6], mrs, msq)
            for dc in range(NDC):
                P.stt(hall[:, dc, tq * TT:(tq + 1) * TT], mxs[:, dc, :],
                      gpre[:, l * NDC + dc:l * NDC + dc + 1], mrs.all(), ALU.mult, ALU.mult)
        P.release(mA)
        if cfg.get("mix_stop") == "A":
            return
        cosT = P.sb("cosT", [128, S], F32)
        sinT = P.sb("sinT", [128, S], F32)
        protb = P.sb("protb", [128, 128], BF16)
        wv = P.sb("wv", [128, NDC, DATT], BF16)
        zs = [P.sb("zs%d" % i, [128, S + 1], F32) for i in range(2)]
        zo = [P.sb("zo%d" % i, [128, S], F32) for i in range(2)]
        qko = [P.sb("qko%d" % i, [128, S], BF16) for i in range(2)]
        zb = [P.sb("zb%d" % i, [128, TT], BF16) for i in range(2)]
        t1 = [P.sb("t1%d" % i, [128, TT], F32) for i in range(2)]
        t2 = [P.sb("t2%d" % i, [128, TT], F32) for i in range(2)]
        vst = [P.sb("vst%d" % i, [128, DATT], BF16) for i in range(2)]
        P.dma("sp", cosT.all(), prm["c_cos"].all())
        P.dma("sp", sinT.all(), prm["c_sin"].all())
        P.dma("pool", protb.all(), prm["c_prot"].all())
        P.dma("pool", wv.all(), wsrc(prm["w_in"], l, 1536, DATT))
        for i in range(2):
            P.memset(zs[i][:, 0:1], 0.0)
        for tb in range(S // 128):
            pa = pb[4 + tb % 2]
            pc = pb[6 + tb % 2]
            for dc in range(NDC):
                P.mm(pa.all(), hall[:, dc, tb * 128:(tb + 1) * 128], wv[:, dc, 0:512],
                     start=(dc == 0), stop=(dc == NDC - 1))
            for dc in range(NDC):
                P.mm(pc[:, 0:256], hall[:, dc, tb * 128:(tb + 1) * 128], wv[:, dc, 512:768],
                     start=(dc == 0), stop=(dc == NDC - 1))
            v_ = vst[tb % 2]
            P.act(v_[:, 0:512], pa.all(), AF.Copy)
            P.copy(v_[:, 512:768], pc[:, 0:256])
            P.dma("sp", VT[tb * 128:(tb + 1) * 128, :], v_.all())
        nq = 0
        nz = 0
        if cfg.get("mix_stop") == "V":
            return
        for ci, (kind, c0, wdt, idx) in enumerate(ZCH):
            if cfg.get("mix_stop") == "B%d" % ci:
                return
            wsl = w_get(base + ci)
            if kind in ("q", "k"):
                ob = qko[nq % 2]
            else:
                ob = zo[nz % 2]
                zsb = zs[nz % 2]
            for tq in range(NTT):
                zp = pb[(ci * NTT + tq) % 2]
                tsl = slice(tq * TT, (tq + 1) * TT)
                for dc in range(NDC):
                    P.mm(zp[0:wdt, :], wsl[:, dc, 0:wdt], hall[:, dc, tsl], start=(dc == 0), stop=(dc == NDC - 1))
                if kind in ("q", "k"):
                    k2 = (ci * NTT + tq) % 2
                    P.act(zb[k2].all(), zp.all(), AF.Copy)
                    rp = pb[2 + k2]
                    P.mm(rp.all(), protb.all(), zb[k2].all())
                    P.tt(t1[k2].all(), zp.all(), cosT[:, tsl], ALU.mult)
                    P.tt(t2[k2].all(), rp.all(), sinT[:, tsl], ALU.mult)
                    P.tt(ob[:, tsl], t1[k2].all(), t2[k2].all(), ALU.add, eng="pool")
                elif kind == "pool":
                    P.act(ob[0:wdt, tsl], zp[0:wdt, :], AF.Copy)
                else:
                    P.act(zsb[0:wdt, 1 + tq * TT:1 + (tq + 1) * TT], zp[0:wdt, :], AF.Copy)
            w_release(base + ci)
            if kind in ("q", "k"):
                P.dma("sp", QK[(0 if kind == "q" else 6) + idx], ob.all())
                nq += 1
            else:
                if kind != "pool":
                    mucol = {"rkv": idx, "zw": 18, "za": 19, "zg": 20 + idx}[kind]
                    P.tt(ob[0:wdt, :], zsb[0:wdt, 0:S], zsb[0:wdt, 1:S + 1], ALU.subtract)
                    P.stt(ob[0:wdt, :], ob[0:wdt, :], pvec[0:wdt, pv0 + mucol:pv0 + mucol + 1],
                          zsb[0:wdt, 1:S + 1], ALU.mult, ALU.add)
                zi = {"rkv": ZS_RKV + idx, "zw": ZS_ZW, "za": ZS_ZA, "zg": ZS_ZG + idx, "pool": ZS_POOL + idx}[kind]
                P.dma("sp", ZS[zi, 0:wdt, :], ob[0:wdt, :])
                nz += 1
        P.release(M0)
        if "att" in cfg.get("mixers", ("att", "rwkv", "pool")):
            attention(l)
            P.release(M0)
        if "pool" in cfg.get("mixers", ("att", "rwkv", "pool")):
            pool_mixer(l)
            P.release(M0)
        if "rwkv" in cfg.get("mixers", ("att", "rwkv", "pool")):
            rwkv(l)
            P.release(M0)
        P.top = FFN_TOP
        gpost = gains["mix_norm_post"]
        ob = base + len(ZCH)
        for tq in range(NTT):
            P.dma("sp", xs.all(), xt_tile(tq))
            P.dma("sp", hb.all(), YM[:, :, tq * TT:(tq + 1) * TT].m(lambda a: a.rearrange("c p t -> p c t")))
            for dc in range(NDC):
                wsl = w_get(ob + tq * NDC + dc)
                yps = pb[4 + dc % 2]
                for fc in range(NDC):
                    P.mm(yps.all(), wsl[:, fc, 0:128], hb[:, fc, :], start=(fc == 0), stop=(fc == NDC - 1))
                w_release(ob + tq * NDC + dc)
                P.act(yb[:, dc, :], yps.all(), AF.Copy)
            post_residual(l, gpost, 1.0, tq)

    def attention(l):
        masks = P.sb("amask", [128, 4, 128], BF16)
        P.dma("pool", masks.all(), prm["c_masks"].all())
        qT = P.sb("qT", [128, S], BF16)
        kT = P.sb("kT", [128, S], BF16)
        V1 = P.sb("V1", [128, 16, 128], BF16)
        V4 = P.sb("V4", [128, 4, 4, 128], BF16)
        V16 = P.sb("V16", [128, 16, 128], BF16)
        NUM = P.sb("NUM", [128, S], F32)
        DEN = P.sb("DEN", [128, S], F32)
        yst = P.sb("yst", [128, S], BF16)
        NP_ = 8
        pt = [P.sb("pt%d" % i, [128, 128], BF16) for i in range(NP_)]
        cnt = {"s": 0, "u": 0}
        for j in range(6):
            P.dma("sp", qT.all(), QK[j])
            P.dma("sp", kT.all(), QK[6 + j])
            cs_ = slice(j * 128, (j + 1) * 128)
            P.dma("sp", V1.all(), VT[:, cs_].m(lambda a: a.rearrange("(kb p) c -> p kb c", p=128)))
            for r in range(4):
                P.dma("sp", V4[:, r, :, :], VT[:, cs_].m(
                    lambda a: a.rearrange("(kb p r) c -> p r kb c", p=128, r=4)[:, r]))
            P.dma("sp", V16.all(), VT[:, cs_].m(lambda a: a.rearrange("(p r) c -> p r c", r=16)))
            units = []
            for (d, vsel) in ((1, lambda r, kb, h: V1[:, kb, h * 64:(h + 1) * 64]),
                              (4, lambda r, kb, h: V4[:, r, kb, h * 64:(h + 1) * 64]),
                              (16, lambda r, kb, h: V16[:, r, h * 64:(h + 1) * 64])):
                nb = S // d // 128
                for r in range(d):
                    for qb in range(nb):
                        units.append((d, r, qb, vsel))
            pend = None

            def toks(d, r, b):
                b0 = d * 128 * b + r
                return slice(b0, b0 + d * 127 + 1, d)

            def stage_s(u):
                d, r, qb, vsel = u
                tl = []
                for h in range(2):
                    hp = slice(h * 64, (h + 1) * 64)
                    lst = [(qb, 2)] + ([(qb - 1, 3)] if qb > 0 else [])
                    for (kb, mi) in lst:
                        k_ = cnt["s"] % NP_
                        cnt["s"] += 1
                        sp_ = pb[k_ // 4][:, (k_ % 4) * 128:(k_ % 4 + 1) * 128]
                        P.mm(sp_, kT[hp, toks(d, r, kb)], qT[hp, toks(d, r, qb)])
                        P.act(pt[k_].all(), sp_, AF.Exp, scale=0.125)
                        P.tt(pt[k_].all(), pt[k_].all(), masks[:, mi, :], ALU.mult, eng="pool")
                        tl.append((h, kb, k_))
                return tl

            def stage_pv(u, tl):
                d, r, qb, vsel = u
                ub = cnt["u"] % 2
                cnt["u"] += 1
                nps = pb[2 + ub][:, 0:128]
                dps = pb[4 + ub][:, 0:128]
                nps_h = lambda h: pb[2 + ub][h * 64:(h + 1) * 64, 0:128]
                dps_h = lambda h: pb[4 + ub][h * 64:(h + 1) * 64, 0:128]
                for h in range(2):
                    hp = slice(h * 64, (h + 1) * 64)
                    mine = [t for t in tl if t[0] == h]
                    for i, (_, kb, k_) in enumerate(mine):
                        P.mm(nps_h(h), vsel(r, kb, h), pt[k_].all(),
                             start=(i == 0), stop=(i == len(mine) - 1))
                for h in range(2):
                    hp = slice(h * 64, (h + 1) * 64)
                    mine = [t for t in tl if t[0] == h]
                    for i, (_, kb, k_) in enumerate(mine):
                        P.mm(dps_h(h), onesb[:, 0:64], pt[k_].all(),
                             start=(i == 0), stop=(i == len(mine) - 1))
                tk = toks(d, r, qb)
                if d == 1:
                    P.act(NUM[:, tk], nps, AF.Copy)
                    P.copy(DEN[:, tk], dps)
                else:
                    P.tt(NUM[:, tk], nps, NUM[:, tk], ALU.add)
                    P.tt(DEN[:, tk], dps, DEN[:, tk], ALU.add)

            for u in units:
                tl = stage_s(u)
                if pend is not None:
                    stage_pv(*pend)
                pend = (u, tl)
            stage_pv(*pend)
            P.recip(DEN.all(), DEN.all())
            P.tt(yst.all(), NUM.all(), DEN.all(), ALU.mult)
            P.dma("sp", YM[j], yst.all())

    def pool_mixer(l):
        pv0 = l * NPV
        pw = P.sb("poolw", [128, 4, 128], BF16)
        fac = P.sb("poolfac", [128, 4, 16], F32)
        P.dma("pool", pw.all(), prm["pool_w"][l].m(lambda a: a.rearrange("g c d -> c g d")))
        P.dma("sp", fac.all(), prm["c_poolfac"].all())
        ub = P.sb("pu", [128, 16 + S], F32)
        sa = P.sb("psa", [128, 16 + S], F32)
        sbb = P.sb("psb", [128, 16 + S], F32)
        pbf = P.sb("ppb", [128, S], BF16)
        t16 = P.sb("pt16", [128, 16], F32)
        yo = P.sb("pyo", [128, S], BF16)
        for b_ in (ub, sa, sbb):
            P.memset(b_[:, 0:16], 0.0)
        for g in range(4):
            P.dma("sp", ub[:, 16:16 + S], ZS[ZS_POOL + g])
            cur = ub
            for k in range(g + 1):
                sh = 1 << k
                nxt = sa if cur is not sa else sbb
                P.tt(nxt[:, 16:16 + S], cur[:, 16:16 + S], cur[:, 16 - sh:16 - sh + S], ALU.add)
                cur = nxt
            w = float(1 << (g + 1))
            P.stt(pbf.all(), cur[:, 16:16 + S], 1.0 / w, ub[:, 16:16 + S], ALU.mult, ALU.subtract)
            P.tt(t16.all(), cur[:, 16:32], fac[:, g, :], ALU.mult)
            P.tt(pbf[:, 0:16], t16.all(), ub[:, 16:32], ALU.subtract)
            for tq in range(NTT):
                yp = pb[tq % 2]
                P.mm(yp.all(), pw[:, g, :], pbf[:, tq * TT:(tq + 1) * TT])
                P.act(yo[:, tq * TT:(tq + 1) * TT], yp.all(), AF.Identity,
                      scale=pvec[:, pv0 + 52 + g:pv0 + 52 + g + 1])
            P.dma("sp", YM[12 + g], yo.all())

    def rwkv(l):
        pv0 = l * NPV
        CH = 64
        NCK = TT // CH
        wupb = P.sb("wupb", [96, 768], BF16)
        aupb = P.sb("aupb", [96, 768], BF16)
        gupb = P.sb("gupb", [128, 2, 768], BF16)
        gnw = P.sb("gnw", [128, 6, 64], F32)
        gnb = P.sb("gnb", [128, 6, 64], F32)
        tmask = P.sb("tmask", [128, 4, 128], BF16)
        e2 = P.sb("e2", [128, 64], BF16)
        bones = P.sb("bones", [128, 128], BF16)
        smask = P.sb("smask", [128, TT], F32)
        P.dma("pool", wupb.all(), prm["rwkv_w_up"][l])
        P.dma("pool", aupb.all(), prm["rwkv_a_up"][l])
        P.dma("pool", gupb.all(), prm["rwkv_g_up"][l].m(lambda a: a.rearrange("(c p) f -> p c f", p=128)))
        P.dma("sp", gnw.all(), prm["gnw"][l].m(lambda a: a.rearrange("p (j v) -> p j v", j=6)))
        P.dma("sp", gnb.all(), prm["gnb"][l].m(lambda a: a.rearrange("p (j v) -> p j v", j=6)))
        P.dma("pool", tmask.all(), prm["c_masks"].all())
        P.dma("pool", e2.all(), prm["c_e2"].all())
        P.dma("pool", bones.all(), prm["c_blockones"].all())
        P.dma("sp", smask.all(), prm["c_scanmask"].all())
        SU, SL, IU = 0, 1, 2
        NG = 3
        rin = P.sb("rin", [128, 9, TT], F32)
        zwf = P.sb("zwf", [96, TT], F32)
        zaf = P.sb("zaf", [96, TT], F32)
        zgf = P.sb("zgf", [128, 2, TT], F32)
        twb = P.sb("twb", [96, TT], BF16)
        zab = P.sb("zab", [96, TT], BF16)
        sgz = P.sb("sgz", [128, 2, TT], BF16)
        f = {nm: P.sb("f_" + nm, [128, TT], F32) for nm in
             ["sig", "a", "cs", "cse", "ei", "en", "ee", "kk", "kkn", "t", "km", "bv"]}
        kk2 = P.sb("kk2", [128, TT], BF16)
        bd = {nm: [P.sb("bd_%s%d" % (nm, jj), [128, NCK, 128], BF16) for jj in range(NG)]
              for nm in ["R", "A", "K", "B", "V", "X"]}
        wc = [P.sb("wc%d" % jj, [128, NCK], F32) for jj in range(NG)]
        gst = P.sb("gst", [128, NCK, NG, 64], F32)
        yoT = P.sb("yoT", [128, NG, S], BF16)
        Mf = P.sb("Mf", [128, NG, 64], F32)
        Mb = P.sb("Mb", [128, NG, 64], BF16)
        NSL = 2
        S4 = [P.sb("S4_%d" % k, [128, NG, 4, 128], BF16) for k in range(NSL)]
        RKT = [P.sb("RKT_%d" % k, [128, NG, 128], BF16) for k in range(NSL)]
        KB = [P.sb("KB_%d" % k, [128, NG, 2, 128], BF16) for k in range(NSL)]
        VST = [P.sb("VST_%d" % k, [128, NG, 64], BF16) for k in range(NSL)]
        BON = [P.sb("BON_%d" % k, [128, NG, 1], F32) for k in range(NSL)]
        QQ = [[P.sb("QQ_%d_%d" % (k, i), [128, NG, 128], BF16) for i in range(2)] for k in range(NSL)]
        PP = [[P.sb("PP_%d_%d" % (k, i), [128, NG, 2, 128], BF16) for i in range(2)] for k in range(NSL)]
        M4 = P.sb("M4", [128, NG, 4, 128], BF16)
        MIU3 = P.sb("MIU3", [128, NG, 128], BF16)
        I3 = P.sb("I3", [128, NG, 128], BF16)
        for jj in range(NG):
            for i_, mi in enumerate((SU, SL, SU, IU)):
                P.copy(M4[:, jj, i_, :], tmask[:, mi, :])
            P.copy(MIU3[:, jj, :], tmask[:, IU, :])
            P.copy(I3[:, jj, :], identb.all())
        Xb = P.sb("Xb", [128, NG, 64], BF16)
        Ub = P.sb("Ub", [128, NG, 64], BF16)
        r2tmp = P.sb("r2tmp", [128, NG, 64], F32)
        r2yn = P.sb("r2yn", [128, NG, 64], F32)
        r2st = P.sb("r2st", [128, NG, 6], F32)
        r2mv = P.sb("r2mv", [128, NG, 2], F32)
        r2rs = P.sb("r2rs", [128, NG], F32)
        YO = P.sb("YO", [128, NG, 128], BF16)
        f3 = lambda vw, n: vw.m(lambda a: a.rearrange("p (j v) -> p j v", j=NG))
        for grp in range(2):
            pairs = [grp * NG + jj for jj in range(NG)]
            for nm in bd:
                for jj in range(NG):
                    P.memset(bd[nm][jj].all(), 0.0, eng="pool")
            P.memset(YO.all(), 0.0)
            P.memset(Mf.all(), 0.0)
            P.memset(Mb.all(), 0.0)
            for tq in range(NTT):
                tsl = slice(tq * TT, (tq + 1) * TT)
                for q3 in range(3):
                    P.dma("sp", rin[:, q3 * 3:(q3 + 1) * 3, :],
                          ZS[ZS_RKV + q3 * 6 + grp * NG:ZS_RKV + q3 * 6 + grp * NG + NG, :, tsl].m(
                              lambda a: a.rearrange("c p t -> p c t")))
                P.dma("sp", zwf.all(), ZS[ZS_ZW, 0:96, tsl])
                P.dma("sp", zaf.all(), ZS[ZS_ZA, 0:96, tsl])
                P.dma("sp", zgf.all(), ZS[ZS_ZG:ZS_ZG + 2, :, tsl].m(lambda a: a.rearrange("c p t -> p c t")))
                P.act(twb.all(), zwf.all(), AF.Tanh)
                P.act(zab.all(), zaf.all(), AF.Copy)
                P.act(sgz.all(), zgf.all(), AF.Sigmoid)
                for jj, j in enumerate(pairs):
                    r_ = rin[:, jj, :]
                    k_ = rin[:, 3 + jj, :]
                    v_ = rin[:, 6 + jj, :]
                    pcol = lambda o: pvec[:, pv0 + o + j:pv0 + o + j + 1]
                    xw = pb[0]
                    P.mm(xw.all(), wupb[:, j * 128:(j + 1) * 128], twb.all())
                    P.act(f["sig"].all(), xw.all(), AF.Sigmoid, bias=pcol(40))
                    ap_ = pb[1]
                    P.mm(ap_.all(), aupb[:, j * 128:(j + 1) * 128], zab.all())
                    P.act(f["a"].all(), ap_.all(), AF.Sigmoid, bias=pcol(34))
                    P.scan(f["cs"].all(), smask.all(), f["sig"].all(), 0.0, ALU.mult, ALU.add)
                    P.tt(f["cse"].all(), f["cs"].all(), f["sig"].all(), ALU.subtract, eng="pool")
                    P.act(f["ei"].all(), f["cs"].all(), AF.Exp, scale=-C05)
                    P.act(f["en"].all(), f["cs"].all(), AF.Exp, scale=C05)
                    P.act(f["ee"].all(), f["cse"].all(), AF.Exp, scale=-C05)
                    P.ts(f["kk"].all(), k_, pcol(22), None, ALU.mult)
                    P.tt(kk2.all(), f["kk"].all(), f["kk"].all(), ALU.mult, eng="pool")
                    ssp = pb[2]
                    P.mm(ssp.all(), bones.all(), kk2.all())
                    P.act(f["kkn"].all(), ssp.all(), AF.Sqrt)
                    P.ts(f["kkn"].all(), f["kkn"].all(), 1e-12, None, ALU.max)
                    P.recip(f["kkn"].all(), f["kkn"].all())
                    P.tt(f["kkn"].all(), f["kkn"].all(), f["kk"].all(), ALU.mult)
                    P.ts(f["t"].all(), f["a"].all(), pcol(28), omka[:, l * 6 + j:l * 6 + j + 1], ALU.mult, ALU.add)
                    P.tt(f["km"].all(), k_, f["t"].all(), ALU.mult, eng="pool")
                    P.tt(f["bv"].all(), f["kkn"].all(), f["a"].all(), ALU.mult, eng="pool")
                    for h in range(2):
                        hp = slice(h * 64, (h + 1) * 64)
                        v3 = lambda vw: vw.m(lambda a: a.rearrange("p (c t) -> p c t", t=CH))
                        dst = lambda nm: bd[nm][jj][hp, :, h * 64:(h + 1) * 64]
                        P.tt(dst("R"), v3(rin[hp, jj, :]), v3(f["ei"][hp, :]), ALU.mult)
                        P.stt(dst("A"), v3(f["kkn"][hp, :]), -1.0, v3(f["ee"][hp, :]), ALU.mult, ALU.mult)
                        P.tt(dst("K"), v3(f["km"][hp, :]), v3(f["en"][hp, :]), ALU.mult, eng="pool")
                        P.tt(dst("B"), v3(f["bv"][hp, :]), v3(f["en"][hp, :]), ALU.mult, eng="pool")
                        P.act(dst("V"), v3(rin[hp, 6 + jj, :]), AF.Copy)
                        P.stt(dst("X"), v3(rin[hp, jj, :]), pvec[hp, pv0 + 46 + j:pv0 + 46 + j + 1],
                              v3(f["km"][hp, :]), ALU.mult, ALU.mult)
                    P.copy(wc[jj].all(), f["ei"][:, CH - 1:TT:CH])
                for c8 in range(NCK):
                    gp = pb[3]
                    for h in range(2):
                        for kc in range(2):
                            hd0 = (2 * pairs[0] + h) * 64
                            rhs = View(gupb.t[:, kc, :].rearrange("p (j x) -> p j x", x=128)[
                                       :, pairs[0]:pairs[0] + NG, h * 64:(h + 1) * 64], gupb,
                                       ((0, 128), (kc, kc + 1), (0, 768)))
                            P.mm(gp[h * 64:(h + 1) * 64, 0:NG * 64],
                                 sgz[:, kc, c8 * CH:(c8 + 1) * CH], rhs, start=(kc == 0), stop=(kc == 1))
                    P.act(gst[:, c8, :, :], gp[:, 0:NG * 64].m(lambda a: a.rearrange("p (j v) -> p j v", v=64)),
                          AF.Copy)

                def R1(c8):
                    k = c8 % NSL
                    B_ = lambda nm, jj: bd[nm][jj][:, c8, :]
                    reg = lambda jj, i, a=0, z=128: pb[2 * jj + i // 4][:, (i % 4) * 128 + a:(i % 4) * 128 + z]
                    for jj in range(NG):
                        P.mm(reg(jj, 0), B_("B", jj), B_("A", jj))
                        P.mm(reg(jj, 1), B_("A", jj), B_("B", jj))
                        P.mm(reg(jj, 2), B_("K", jj), B_("A", jj))
                        P.mm(reg(jj, 3), B_("B", jj), B_("R", jj))
                        P.mm(reg(jj, 4), B_("K", jj), B_("R", jj))
                        P.mm(reg(jj, 5), B_("K", jj), identb.all())
                        P.mm(reg(jj, 6), B_("B", jj), identb.all())
                        P.mm(reg(jj, 7, 0, 64), B_("V", jj), e2.all())
                        P.mm(reg(jj, 7, 64, 65), B_("X", jj), onesb[:, 0:1])
                    yield
                    fl = lambda vw: vw.m(lambda a: a.rearrange("p j r c -> p j (r c)"))
                    P.tt(fl(S4[k].all()), PSF[:, 0:6:2, :], fl(M4.all()), ALU.mult)
                    P.tt(RKT[k].all(), PSF[:, 1:7:2, 0:128], MIU3.all(), ALU.mult)
                    P.act(fl(KB[k].all()), PSF[:, 1:7:2, 128:384], AF.Copy)
                    P.act(VST[k].all(), PSF[:, 1:7:2, 384:448], AF.Copy)
                    P.act(BON[k].all(), PSF[:, 1:7:2, 448:449], AF.Copy)
                    P.tt(QQ[k][0].all(), S4[k][:, :, 0, :], I3.all(), ALU.add)
                    yield
                    for lev in range(1, 7):
                        if lev == 1:
                            Pc = lambda jj: S4[k][:, jj, 0, :]
                            PTc = lambda jj: S4[k][:, jj, 1, :]
                        else:
                            Pc = (lambda pp: (lambda jj: pp[:, jj, 0, :]))(PP[k][lev % 2])
                            PTc = (lambda pp: (lambda jj: pp[:, jj, 1, :]))(PP[k][lev % 2])
                        PPn = PP[k][(lev + 1) % 2]
                        Qc, Qn = QQ[k][lev % 2], QQ[k][(lev + 1) % 2]
                        for jj in range(NG):
                            if lev <= 4:
                                P.mm(reg(jj, 0), PTc(jj), Pc(jj))
                            if lev <= 5:
                                P.mm(reg(jj, 1), Pc(jj), PTc(jj))
                            if lev >= 2:
                                P.mm(reg(jj, 2), PTc(jj), Qc[:, jj, :])
                        yield
                        if lev <= 4:
                            P.act(PPn.all().m(lambda a: a.rearrange("p j r c -> p j (r c)")),
                                  PSF[:, 0:6:2, 0:256], AF.Copy)
                        elif lev == 5:
                            P.act(PPn[:, :, 1, :], PSF[:, 0:6:2, 128:256], AF.Copy)
                        if lev >= 2:
                            P.tt(Qn.all(), PSF[:, 0:6:2, 256:384], Qc.all(), ALU.add)
                        yield

                def R2(c8):
                    k = c8 % NSL
                    B_ = lambda nm, jj: bd[nm][jj][:, c8, :]
                    xr = lambda jj: pb[6][:, jj * 64:(jj + 1) * 64]
                    ur = lambda jj: pb[6][:, 192 + jj * 64:192 + (jj + 1) * 64]
                    yr = lambda jj: pb[7][:, jj * 64:(jj + 1) * 64]
                    dr = lambda jj: pb[7][:, 192 + jj * 64:192 + (jj + 1) * 64]
                    for jj in range(NG):
                        P.mm(xr(jj), B_("A", jj), Mb[:, jj, :], start=True, stop=False)
                        P.mm(xr(jj), S4[k][:, jj, 2, :], VST[k][:, jj, :], start=False, stop=True)
                    yield
                    P.act(Xb.all(), f3(pb[6][:, 0:192], NG), AF.Copy)
                    yield
                    for jj in range(NG):
                        P.mm(ur(jj), QQ[k][1][:, jj, :], Xb[:, jj, :])
                    yield
                    P.act(Ub.all(), f3(pb[6][:, 192:384], NG), AF.Copy)
                    yield
                    for jj in range(NG):
                        P.mm(yr(jj), B_("R", jj), Mb[:, jj, :], start=True, stop=False)
                        P.mm(yr(jj), S4[k][:, jj, 3, :], Ub[:, jj, :], start=False, stop=False)
                        P.mm(yr(jj), RKT[k][:, jj, :], VST[k][:, jj, :], start=False, stop=True)
                    for jj in range(NG):
                        P.mm(dr(jj), KB[k][:, jj, 1, :], Ub[:, jj, :], start=True, stop=False)
                        P.mm(dr(jj), KB[k][:, jj, 0, :], VST[k][:, jj, :], start=False, stop=True)
                    yield
                    P.tt(r2tmp.all(), f3(pb[7][:, 192:384], NG), Mf.all(), ALU.add)
                    for jj in range(NG):
                        P.ts(Mf[:, jj, :], r2tmp[:, jj, :], wc[jj][:, c8:c8 + 1], None, ALU.mult)
                    P.act(Mb.all(), Mf.all(), AF.Copy)
                    for jj in range(NG):
                        P.bn_stats(r2st[:, jj, :], yr(jj))
                        P.bn_aggr(r2mv[:, jj, :], r2st[:, jj, :])
                    P.act(r2rs.all(), r2mv[:, :, 1], AF.Sqrt, bias=gneps.all())
                    P.recip(r2rs.all(), r2rs.all())
                    for jj in range(NG):
                        P.ts(r2yn[:, jj, :], yr(jj), r2mv[:, jj, 0:1], r2rs[:, jj:jj + 1], ALU.subtract, ALU.mult)
                    P.tt(r2yn.all(), r2yn.all(), gnw[:, grp * NG:(grp + 1) * NG, :], ALU.mult)
                    P.tt(r2yn.all(), r2yn.all(), gnb[:, grp * NG:(grp + 1) * NG, :], ALU.add)
                    for jj in range(NG):
                        P.stt(r2yn[:, jj, :], VST[k][:, jj, :], BON[k][:, jj, :], r2yn[:, jj, :], ALU.mult, ALU.add)
                    for h in range(2):
                        hp = slice(h * 64, (h + 1) * 64)
                        P.tt(YO[hp, :, h * 64:(h + 1) * 64], r2yn[hp, :, :], gst[hp, c8, :, :], ALU.mult)
                    yield
                    for jj in range(NG):
                        P.mm(xr(jj), YO[:, jj, :], e2.all())
                    yield
                    P.act(yoT[:, :, tq * TT + c8 * CH:tq * TT + (c8 + 1) * CH], f3(pb[6][:, 0:192], NG), AF.Copy)

                for step in range(NCK + 1):
                    gens = []
                    if step < NCK:
                        gens.append(R1(step))
                    if step > 0:
                        gens.append(R2(step - 1))
                    rr(gens)
            for jj, j in enumerate(pairs):
                P.dma("sp", YM[6 + j], yoT[:, jj, :])

    input_transpose()
    phases = cfg.get("phases", ("ffn1", "mix", "ffn2"))
    plan = []
    for l in range(n_layers):
        for ph in phases:
            plan.append((l, ph))
    bases = {}
    for (l, ph) in plan:
        if ph in ("ffn1", "ffn2"):
            which = 1 if ph == "ffn1" else 2
            for tq in range(NTT):
                bases[(l, ph, tq)] = ffn_enqueue(l, which)
        else:
            bases[(l, ph)] = mix_enqueue(l)
    for (l, ph) in plan:
        if ph in ("ffn1", "ffn2"):
            which = 1 if ph == "ffn1" else 2
            P.top = FFN_TOP
            for tq in range(NTT):
                ffn_tile(l, which, tq, bases[(l, ph, tq)])
        else:
            mixer(l, bases[(l, ph)])
    P.top = FFN_TOP
    output_transpose()
    for k in list(wstate["slots"]):
        P.s.final.append(("pool", wstate["dix"][k]))
    P.s.emit(nc, es)
    es.close()
    return nc


def host_consts():
    c = {}
    c["c_ident"] = np.eye(128, dtype=np.float32)
    prot = np.zeros((128, 128), np.float32)
    for m in range(128):
        if m % 64 < 32:
            prot[m + 32, m] = -1.0
        else:
            prot[m - 32, m] = 1.0
    c["c_prot"] = prot
    inv = (10000.0 ** (-np.arange(0, 64, 2, dtype=np.float32) / 64)).astype(np.float32)
    ang = np.arange(S, dtype=np.float32)[:, None] * inv[None, :]
    cs, sn = np.cos(ang).astype(np.float32), np.sin(ang).astype(np.float32)
    rows = np.arange(128) % 32
    c["c_cos"] = np.ascontiguousarray(cs.T[rows])
    c["c_sin"] = np.ascontiguousarray(sn.T[rows])
    p = np.arange(128)[:, None]
    f_ = np.arange(128)[None, :]
    c["c_masks"] = np.ascontiguousarray(
        np.stack([(p < f_), (p > f_), (p <= f_), (p >= f_)], axis=1).astype(np.float32))
    c["c_e2"] = np.concatenate([np.eye(64, dtype=np.float32)] * 2, axis=0)
    sm = np.ones((128, 512), np.float32)
    sm[:, ::64] = 0.0
    c["c_scanmask"] = sm
    pf = np.zeros((128, 4, 16), np.float32)
    for g in range(4):
        w = 2 ** (g + 1)
        pf[:, g, :] = 1.0 / np.minimum(np.arange(1, 17), w)
    c["c_poolfac"] = pf
    c["c_blockones"] = ((p // 64) == (f_ // 64)).astype(np.float32)
    return c


def make_in_map(inputs, b, consts=None):
    m = {"x": np.ascontiguousarray(inputs["x"][b])}
    for nm in ["ffn1_norm_pre", "ffn1_norm_post", "mix_norm_pre", "mix_norm_post",
               "ffn2_norm_pre", "ffn2_norm_post"]:
        m[nm] = np.ascontiguousarray(inputs[nm]).reshape(L * NDC, 128)
    for nm in ["ffn1_w_gate", "ffn1_w_up", "ffn1_w_down", "w_in", "w_out",
               "ffn2_w_gate", "ffn2_w_up", "ffn2_w_down", "rwkv_w_up", "rwkv_a_up", "rwkv_g_up", "pool_w"]:
        m[nm] = np.ascontiguousarray(inputs[nm])
    pv = np.zeros((L, NPV, 128), np.float32)
    mu = inputs["rwkv_mu"]
    pv[:, 0:18, :] = mu[:, 0:2304].reshape(L, 18, 128)
    pv[:, 18, 0:96] = mu[:, 2304:2400]
    pv[:, 19, 0:96] = mu[:, 2400:2496]
    pv[:, 20:22, :] = mu[:, 2496:2752].reshape(L, 2, 128)
    pv[:, 22:28, :] = inputs["rwkv_k_k"].reshape(L, 6, 128)
    pv[:, 28:34, :] = inputs["rwkv_k_a"].reshape(L, 6, 128)
    pv[:, 34:40, :] = inputs["rwkv_a0"].reshape(L, 6, 128)
    pv[:, 40:46, :] = inputs["rwkv_w0"].reshape(L, 6, 128)
    pv[:, 46:52, :] = inputs["rwkv_r_k"].reshape(L, 6, 128)
    pv[:, 52:56, :] = inputs["pool_scale"].reshape(L, 4, 128)
    m["pvec"] = pv.reshape(L * NPV, 128)
    for nm, key in (("gnw", "rwkv_gn_w"), ("gnb", "rwkv_gn_b")):
        g = inputs[key].reshape(L, 6, 2, 64)
        g = np.transpose(g, (0, 2, 1, 3))
        m[nm] = np.ascontiguousarray(np.repeat(g, 64, axis=1).reshape(L, 128, 384))
    m.update(consts if consts is not None else host_consts())
    return m


def kernel(**inputs):
    inputs = {k: np.asarray(v, dtype=np.float32) for k, v in inputs.items()}
    nc = build({})
    consts = host_consts()
    in_maps = [make_in_map(inputs, b, consts) for b in range(8)]
    res = run_bass_kernel_spmd(nc, in_maps, core_ids=list(range(8)))
    return np.stack([r["out"] for r in res.results], axis=0).astype(np.float32)
```

```python
import numpy as np
from contextlib import ExitStack
import concourse.bass as bass
import concourse.mybir as mybir
from concourse.bass_utils import run_bass_kernel_spmd

F32 = mybir.dt.float32
BF16 = mybir.dt.bfloat16
AF = mybir.ActivationFunctionType
ALU = mybir.AluOpType

D = 2048
S = 2048
L = 4
DFF = 5632
NFC = DFF // 128
NDC = D // 128
TT = 512
NTT = S // TT
DATT = 768
DRW = 768
DPOOL = 512
DIN = 5568
RW_IN = 2752
EPS = 1e-6
GN_EPS = 64e-5


class Buf:
    def __init__(self, t, shape, name):
        self.t = t
        self.shape = tuple(shape)
        self.name = name

    def __getitem__(self, idx):
        if not isinstance(idx, tuple):
            idx = (idx,)
        box = []
        for d, n in enumerate(self.shape):
            if d < len(idx):
                i = idx[d]
                if isinstance(i, slice):
                    lo = 0 if i.start is None else i.start
                    hi = n if i.stop is None else i.stop
                    box.append((lo, hi))
                else:
                    box.append((i, i + 1))
            else:
                box.append((0, n))
        v = View(self.t[idx], self, tuple(box))
        if getattr(self, "psum", False):
            if getattr(self, "bank", None) is not None:
                v.banks = (self.bank,)
            else:
                i1 = idx[1] if len(idx) > 1 else slice(None)
                if isinstance(i1, slice):
                    v.banks = tuple(range(*i1.indices(self.shape[1])))
                else:
                    v.banks = (i1,)
        return v

    def all(self):
        return self[tuple(slice(None) for _ in self.shape)]


class View:
    banks = None

    def __init__(self, ap, buf, box):
        self.ap = ap
        self.buf = buf
        self.box = box

    def m(self, fn):
        v = View(fn(self.ap), self.buf, self.box)
        v.banks = self.banks
        return v


def _overlap(a, b):
    for (l0, h0), (l1, h1) in zip(a, b):
        if h0 <= l1 or h1 <= l0:
            return False
    return True


def _covers(a, b):
    for (l0, h0), (l1, h1) in zip(a, b):
        if l0 > l1 or h0 < h1:
            return False
    return True


class Op:
    __slots__ = ("eng", "fn", "deps", "dma", "waits", "signal", "pos", "sem", "val", "clock")

    def __init__(self, eng, fn, deps, dma):
        self.eng = eng
        self.fn = fn
        self.deps = deps
        self.dma = dma
        self.waits = []
        self.signal = False
        self.sem = None
        self.val = None


class Sched:
    ENGS = ("pe", "act", "dve", "pool", "sp")
    NDMA_SEM = 40
    EPOCH = 8000

    def __init__(self):
        self.ops = []
        self.track = {}
        self.final = []
        self.bar = None
        self.last = {}
        self.dmas = []
        self.psum_last = {}

    def add(self, eng, fn, reads=(), writes=(), dma=False):
        i = len(self.ops)
        deps = set()
        for v in list(reads) + list(writes):
            if v.banks is not None:
                for bk in v.banks:
                    lb = self.psum_last.setdefault(bk, {})
                    for f_, j_ in lb.items():
                        if f_ != eng:
                            deps.add(j_)
                    lb[eng] = i
        reads = [v for v in reads if v.banks is None]
        writes = [v for v in writes if v.banks is None]
        for v in reads:
            ents = self.track.setdefault(v.buf.name, [])
            for e in ents:
                if _overlap(e[0], v.box):
                    if e[1] is not None:
                        deps.add(e[1])
                    rd = e[2]
                    if dma:
                        if i not in rd:
                            rd.append(i)
                    else:
                        for k in range(len(rd)):
                            if rd[k] == i:
                                break
                            o = self.ops[rd[k]]
                            if (not o.dma) and o.eng == eng:
                                rd[k] = i
                                break
                        else:
                            rd.append(i)
        for v in writes:
            ents = self.track.setdefault(v.buf.name, [])
            keep = []
            for e in ents:
                if _overlap(e[0], v.box):
                    if e[1] is not None:
                        deps.add(e[1])
                    deps.update(e[2])
                    if _covers(v.box, e[0]):
                        continue
                keep.append(e)
            keep.append([v.box, i, []])
            self.track[v.buf.name] = keep
        deps.discard(i)
        if self.bar is not None:
            deps.add(self.bar)
        self.ops.append(Op(eng, fn, deps, dma))
        if dma:
            self.dmas.append(i)
        else:
            self.last[eng] = i
        return i

    def barrier(self, fn):
        i = len(self.ops)
        deps = set(self.last.values()) | set(self.dmas)
        self.ops.append(Op("dve", fn, deps, False))
        self.last["dve"] = i
        self.dmas = []
        self.bar = i
        return i

    def plan(self):
        clock = {e: {} for e in self.ENGS}
        pos = {e: 0 for e in self.ENGS}
        dma_rr = 0
        dma_cnt = [0] * self.NDMA_SEM
        dma_last = [None] * self.NDMA_SEM
        ops = self.ops
        for i, op in enumerate(ops):
            E = op.eng
            ck = clock[E]
            newck = None
            waits = []
            need = sorted(op.deps, reverse=True)
            if op.dma:
                s = dma_rr % self.NDMA_SEM
                dma_rr += 1
                dma_cnt[s] += 1
                op.sem = ("d", s)
                op.val = 16 * dma_cnt[s]
                if dma_last[s] is not None:
                    need.append(dma_last[s])
                dma_last[s] = i
            else:
                op.pos = pos[E]
                pos[E] += 1
            for j in need:
                oj = ops[j]
                if oj.dma:
                    key, val = oj.sem, oj.val
                else:
                    if oj.eng == "pe" and E == "pe" and not op.dma:
                        continue
                    key, val = ("c", oj.eng), oj.pos
                cur = ck if newck is None else newck
                if cur.get(key, -1) >= val:
                    continue
                if newck is None:
                    newck = dict(ck)
                for k2, v2 in oj.clock.items():
                    if newck.get(k2, -1) < v2:
                        newck[k2] = v2
                waits.append(j)
                oj.signal = True
            if newck is not None:
                clock[E] = newck
                ck = newck
            op.waits = waits
            c = dict(ck)
            if op.dma:
                c[op.sem] = op.val
            else:
                c[("c", E)] = op.pos
            op.clock = c
        for op in ops:
            op.clock = None

    def emit(self, nc, es):
        self.plan()
        ops = self.ops
        cnt = {e: 0 for e in self.ENGS}
        nep = {e: 1 for e in self.ENGS}
        for op in ops:
            if op.dma or not op.signal:
                continue
            E = op.eng
            cnt[E] += 1
            ep = (cnt[E] - 1) // self.EPOCH
            op.sem = ("c", E, ep)
            op.val = cnt[E] - ep * self.EPOCH
            nep[E] = max(nep[E], ep + 1)
        sems = {}
        for E in self.ENGS:
            for ep in range(nep[E]):
                sems[("c", E, ep)] = es.enter_context(nc.semaphore("s_%s_%d" % (E, ep)))
        for s in range(self.NDMA_SEM):
            sems[("d", s)] = es.enter_context(nc.semaphore("s_dma_%d" % s))
        per = {e: [] for e in self.ENGS}
        for i, op in enumerate(ops):
            per[op.eng].append(op)
        final = self.final
        block = es.enter_context(nc.Block())

        def run(E, eng):
            for op in per[E]:
                w = {}
                for j in op.waits:
                    oj = ops[j]
                    if w.get(oj.sem, -1) < oj.val:
                        w[oj.sem] = oj.val
                for k, v in w.items():
                    eng.wait_ge(sems[k], v)
                ins = op.fn(eng)
                if op.dma:
                    ins.then_inc(sems[op.sem], 16)
                elif op.signal:
                    ins.then_inc(sems[op.sem], 1)
            for (fe, j) in final:
                if fe == E:
                    oj = ops[j]
                    eng.wait_ge(sems[oj.sem], oj.val)

        @block.tensor
        def _(e):
            run("pe", e)

        @block.scalar
        def _(e):
            run("act", e)

        @block.vector
        def _(e):
            run("dve", e)

        @block.gpsimd
        def _(e):
            run("pool", e)

        @block.sync
        def _(e):
            run("sp", e)


U8 = mybir.dt.uint8
ISZ = {F32: 4, BF16: 2}
ARENA = 206 * 1024


class SBuf:
    def __init__(self, arena_t, off, shape, dt, name):
        self.name = name
        self.shape = tuple(shape)
        self.isz = ISZ[dt]
        n = 1
        for d in shape[1:]:
            n *= d
        self.nbytes = n * self.isz
        ap = arena_t[:, off:off + self.nbytes].bitcast(dt)
        if len(shape) == 3:
            ap = ap.rearrange("p (a b) -> p a b", a=shape[1])
        elif len(shape) == 4:
            ap = ap.rearrange("p (a b c) -> p a b c", a=shape[1], b=shape[2])
        if shape[0] < 128:
            ap = ap[0:shape[0]]
        self.t = ap

    __getitem__ = Buf.__getitem__
    all = Buf.all


class Prog:
    def __init__(self, nc, es, cfg):
        self.nc = nc
        self.es = es
        self.cfg = cfg
        self.s = Sched()
        self.arena = es.enter_context(nc.sbuf_tensor("arena", [128, ARENA], U8))
        self.top = 0
        self.nname = 0
        self.barsc = self.sb("barsc", [128, 8], F32)

    def sb(self, name, shape, dt):
        n = 1
        for d in shape[1:]:
            n *= d
        nb = (n * ISZ[dt] + 63) // 64 * 64
        assert self.top + nb <= ARENA, "SBUF arena overflow at %s: %d" % (name, self.top + nb)
        self.nname += 1
        b = SBuf(self.arena, self.top, shape, dt, "%s#%d" % (name, self.nname))
        self.top += nb
        return b

    def mark(self):
        return self.top

    def release(self, m):
        self.barrier()
        self.top = m

    def barrier(self):
        ap = self.barsc.all().ap
        self.s.barrier(lambda e: e.memset(ap, 0.0))

    def ps_all(self):
        t = self.es.enter_context(self.nc.psum_tensor("PS", [128, 8, 512], F32))
        full = Buf(t, (128, 8, 512), "PS")
        full.psum = True
        full.bank = None
        banks = []
        for i in range(8):
            bk = Buf(t[:, i, :], (128, 512), "pb%d" % i)
            bk.psum = True
            bk.bank = i
            banks.append(bk)
        return full, banks

    def dram(self, name, shape, dt, kind="Internal"):
        t = self.nc.dram_tensor(name, list(shape), dt, kind=kind)
        return Buf(t, shape, name)

    def mm(self, out, lhsT, rhs, start=True, stop=True, **kw):
        self.s.add("pe", lambda e: e.matmul(out.ap, lhsT.ap, rhs.ap, start=start, stop=stop, **kw),
                   reads=[lhsT, rhs], writes=[out])

    def transpose(self, out, in_, ident):
        self.s.add("pe", lambda e: e.transpose(out.ap, in_.ap, ident.ap),
                   reads=[in_, ident], writes=[out])

    def act(self, out, in_, func, scale=1.0, bias=None, accum=None):
        rd = [in_]
        kw = {}
        if isinstance(scale, View):
            rd.append(scale)
            kw["scale"] = scale.ap
        else:
            kw["scale"] = scale
        if isinstance(bias, View):
            rd.append(bias)
            kw["bias"] = bias.ap
        elif bias is not None:
            kw["bias"] = bias
        wr = [out]
        if accum is not None:
            kw["accum_out"] = accum.ap
            wr.append(accum)
        self.s.add("act", lambda e: e.activation(out=out.ap, in_=in_.ap, func=func, **kw),
                   reads=rd, writes=wr)

    def tt(self, out, a, b, op, eng="dve"):
        eng = "dve"
        self.s.add(eng, lambda e: e.tensor_tensor(out.ap, a.ap, b.ap, op), reads=[a, b], writes=[out])

    def ts(self, out, a, s1, s2=None, op0=ALU.mult, op1=None, eng="dve"):
        eng = "dve"
        rd = [a]
        v1 = s1
        v2 = s2
        if isinstance(s1, View):
            rd.append(s1)
            v1 = s1.ap
        if isinstance(s2, View):
            rd.append(s2)
            v2 = s2.ap
        if op1 is None:
            self.s.add(eng, lambda e: e.tensor_scalar(out.ap, a.ap, v1, None, op0), reads=rd, writes=[out])
        else:
            self.s.add(eng, lambda e: e.tensor_scalar(out.ap, a.ap, v1, v2, op0, op1), reads=rd, writes=[out])

    def stt(self, out, in0, scalar, in1, op0, op1):
        rd = [in0, in1]
        sv = scalar
        if isinstance(scalar, View):
            rd.append(scalar)
            sv = scalar.ap
        self.s.add("dve", lambda e: e.scalar_tensor_tensor(out.ap, in0.ap, sv, in1.ap, op0, op1),
                   reads=rd, writes=[out])

    def copy(self, out, in_, eng="dve"):
        eng = "dve"
        self.s.add(eng, lambda e: e.tensor_copy(out.ap, in_.ap), reads=[in_], writes=[out])

    def recip(self, out, in_):
        self.s.add("dve", lambda e: e.reciprocal(out.ap, in_.ap), reads=[in_], writes=[out])

    def memset(self, out, val, eng="dve"):
        eng = "dve"
        self.s.add(eng, lambda e: e.memset(out.ap, val), reads=[], writes=[out])

    def scan(self, out, d0, d1, init, op0, op1):
        self.s.add("dve", lambda e: e.tensor_tensor_scan(out.ap, d0.ap, d1.ap, init, op0, op1),
                   reads=[d0, d1], writes=[out])

    def bn_stats(self, out, in_):
        self.s.add("dve", lambda e: e.bn_stats(out.ap, in_.ap), reads=[in_], writes=[out])

    def bn_aggr(self, out, in_):
        self.s.add("dve", lambda e: e.bn_aggr(out.ap, in_.ap), reads=[in_], writes=[out])

    def dma(self, eng, out, in_):
        return self.s.add(eng, lambda e: e.dma_start(out=out.ap, in_=in_.ap), reads=[in_], writes=[out], dma=True)


def rr(gens):
    gens = list(gens)
    while gens:
        nxt = []
        for g in gens:
            try:
                next(g)
                nxt.append(g)
            except StopIteration:
                pass
        gens = nxt


NPV = 56
C05 = float(np.exp(-0.5))
ZCH = ([("q", i * 128, 128, i) for i in range(6)] + [("k", 768 + i * 128, 128, i) for i in range(6)]
       + [("rkv", 2304 + i * 128, 128, i) for i in range(18)]
       + [("zw", 4608, 96, 0), ("za", 4704, 96, 0), ("zg", 4800, 128, 0), ("zg", 4928, 128, 1)]
       + [("pool", 5056 + i * 128, 128, i) for i in range(4)])
ZS_RKV, ZS_ZW, ZS_ZA, ZS_ZG, ZS_POOL = 0, 18, 19, 20, 22
NZS = 26


def build(cfg):
    nc = bass.Bass("TRN2", target_bir_lowering=False)
    es = ExitStack()
    P = Prog(nc, es, cfg)
    n_layers = cfg.get("n_layers", L)
    dbg = cfg.get("dbg", False)

    def ext(name, shape):
        return P.dram(name, shape, F32, kind="ExternalInput")

    x_in = ext("x", [S, D])
    prm = {}
    for nm, shp in [
        ("ffn1_norm_pre", [L * NDC, 128]), ("ffn1_norm_post", [L * NDC, 128]),
        ("ffn1_w_gate", [L, D, DFF]), ("ffn1_w_up", [L, D, DFF]), ("ffn1_w_down", [L, DFF, D]),
        ("mix_norm_pre", [L * NDC, 128]), ("mix_norm_post", [L * NDC, 128]),
        ("w_in", [L, D, DIN]), ("w_out", [L, D, D]),
        ("ffn2_norm_pre", [L * NDC, 128]), ("ffn2_norm_post", [L * NDC, 128]),
        ("ffn2_w_gate", [L, D, DFF]), ("ffn2_w_up", [L, D, DFF]), ("ffn2_w_down", [L, DFF, D]),
        ("rwkv_w_up", [L, 96, 768]), ("rwkv_a_up", [L, 96, 768]), ("rwkv_g_up", [L, 256, 768]),
        ("pool_w", [L, 4, 128, 128]), ("pvec", [L * NPV, 128]),
        ("gnw", [L, 128, 384]), ("gnb", [L, 128, 384]),
        ("c_ident", [128, 128]), ("c_prot", [128, 128]), ("c_cos", [128, S]), ("c_sin", [128, S]),
        ("c_masks", [128, 4, 128]), ("c_e2", [128, 64]), ("c_scanmask", [128, 512]),
        ("c_poolfac", [128, 4, 16]), ("c_blockones", [128, 128]),
    ]:
        prm[nm] = ext(nm, shp)
    out_d = P.dram("out", [S, D], F32, kind="ExternalOutput")
    XT = P.dram("XT", [NDC, 128, S], F32)
    QK = P.dram("QK", [12, 128, S], BF16)
    VT = P.dram("VT", [S, DATT], BF16)
    ZS = P.dram("ZS", [NZS, 128, S], F32)
    YM = P.dram("YM", [NDC, 128, S], BF16, kind=("ExternalOutput" if dbg else "Internal"))

    ident = P.sb("identf", [128, 128], F32)
    identb = P.sb("identb", [128, 128], BF16)
    ones = P.sb("onesf", [128, 128], F32)
    onesb = P.sb("onesb", [128, 128], BF16)
    epsb = P.sb("epsb", [128, 1], F32)
    gneps = P.sb("gneps", [128, 1], F32)
    gains = {}
    for nm in ["ffn1_norm_pre", "ffn1_norm_post", "mix_norm_pre", "mix_norm_post",
               "ffn2_norm_pre", "ffn2_norm_post"]:
        gains[nm] = P.sb("g_" + nm, [128, L * NDC], F32)
    pvec = P.sb("pvec", [128, L * NPV], F32)
    omka = P.sb("omka", [128, L * 6], F32)
    NWA = 4
    NWD = 3
    NQF = NFC // 4
    wA = [P.sb("wA%d" % i, [128, NDC, 256], BF16) for i in range(NWA)]
    PSF, pb = P.ps_all()
    M0 = P.mark()

    xs = P.sb("xs", [128, NDC, TT], F32)
    hb = P.sb("hb", [128, NDC, TT], BF16)
    actb = P.sb("actb", [128, NFC, TT], BF16)
    yb = P.sb("yb", [128, NDC, TT], F32)
    sq = [P.sb("sq%d" % i, [128, TT], F32) for i in range(2)]
    sg = sq
    rstd = P.sb("rstd", [128, TT], F32)
    gstage = P.sb("gstage", [128, 128], F32)
    wD = [P.sb("wD%d" % i, [128, NQF, 512], BF16) for i in range(NWD)]
    FFN_TOP = P.mark()

    for bk in pb:
        P.memset(bk.all(), 0.0)
    P.dma("sp", ident.all(), prm["c_ident"].all())
    P.dma("pool", identb.all(), prm["c_ident"].all())
    P.memset(ones.all(), 1.0)
    P.memset(onesb.all(), 1.0)
    P.memset(epsb.all(), EPS)
    P.memset(gneps.all(), GN_EPS)
    for gi, nm in enumerate(gains):
        P.dma("sp", gstage[0:L * NDC, :], prm[nm].all())
        P.transpose(pb[7][:, 0:L * NDC], gstage[0:L * NDC, :], ident[0:L * NDC, 0:L * NDC])
        P.copy(gains[nm].all(), pb[7][:, 0:L * NDC])
    for l in range(L):
        P.dma("sp", gstage[0:NPV, :], prm["pvec"][l * NPV:(l + 1) * NPV, :])
        P.transpose(pb[7][:, 0:NPV], gstage[0:NPV, :], ident[0:NPV, 0:NPV])
        P.copy(pvec[:, l * NPV:(l + 1) * NPV], pb[7][:, 0:NPV])
        P.ts(omka[:, l * 6:(l + 1) * 6], pvec[:, l * NPV + 28:l * NPV + 34], -1.0, 1.0, ALU.mult, ALU.add)

    wq = []
    NSLOT = {"A": NWA, "D": NWD}
    wstate = {"issued": 0, "slots": {}, "rel": {"A": 0, "D": 0}, "cnt": {"A": 0, "D": 0}}

    def w_ensure(k):
        while wstate["issued"] < min(k + 1, len(wq)):
            i = wstate["issued"]
            cls, src, wdt = wq[i]
            if wstate["cnt"][cls] - wstate["rel"][cls] >= NSLOT[cls]:
                break
            slot = (wA if cls == "A" else wD)[wstate["cnt"][cls] % NSLOT[cls]]
            wstate["cnt"][cls] += 1
            wstate["slots"][i] = slot
            wstate.setdefault("dix", {})[i] = P.dma("pool", slot[:, :, 0:wdt], src)
            wstate["issued"] += 1

    def w_get(k, depth=4):
        w_ensure(k + depth)
        assert k in wstate["slots"], "weight %d not issued" % k
        return wstate["slots"][k]

    def w_release(k):
        wstate["rel"][wq[k][0]] += 1
        del wstate["slots"][k]

    def wsrc(buf, l, c0, wdt):
        return buf[l, :, c0:c0 + wdt].m(lambda a: a.rearrange("(c p) f -> p c f", p=128))

    def xt_tile(tq):
        return XT[:, :, tq * TT:(tq + 1) * TT].m(lambda a: a.rearrange("c p t -> p c t"))

    def input_transpose():
        for tb in range(S // 128):
            P.dma("sp", xs[:, 0:4, :],
                  x_in[tb * 128:(tb + 1) * 128, :].m(lambda a: a.rearrange("p (c f) -> p c f", c=4)))
            for g4 in range(4):
                bank = pb[(tb * 4 + g4) % 4]
                for j in range(4):
                    dc = g4 * 4 + j
                    P.transpose(bank[:, j * 128:(j + 1) * 128],
                                xs[:, dc // 4, (dc % 4) * 128:(dc % 4 + 1) * 128], ident.all())
                st = yb[:, (tb * 4 + g4) % NDC, :]
                if (tb * 4 + g4) % 2 == 0:
                    P.copy(st, bank.all())
                else:
                    P.act(st, bank.all(), AF.Copy)
                P.dma("sp", XT[g4 * 4:(g4 + 1) * 4, :, tb * 128:(tb + 1) * 128].m(
                    lambda a: a.rearrange("c p t -> p c t")),
                    st.m(lambda a: a.rearrange("p (c t) -> p c t", c=4)))

    def output_transpose():
        for tq in range(NTT):
            P.dma("sp", xs.all(), xt_tile(tq))
            for tb in range(TT // 128):
                for g4 in range(4):
                    bank = pb[(tb * 4 + g4) % 4]
                    for j in range(4):
                        dc = g4 * 4 + j
                        P.transpose(bank[:, j * 128:(j + 1) * 128],
                                    xs[:, dc, tb * 128:(tb + 1) * 128], ident.all())
                    st = yb[:, (tb * 4 + g4) % NDC, :]
                    if (tb * 4 + g4) % 2 == 0:
                        P.copy(st, bank.all())
                    else:
                        P.act(st, bank.all(), AF.Copy)
                    r0 = tq * TT + tb * 128
                    di = P.dma("sp", out_d[r0:r0 + 128, g4 * 512:(g4 + 1) * 512], st)
                    P.s.final.append(("sp", di))

    def rms_rstd(src_of_dc, stat_bank, rs, sqs):
        for dc in range(NDC):
            t = sqs[dc % 2]
            P.act(t.all(), src_of_dc(dc), AF.Square)
            P.mm(stat_bank.all(), ones.all(), t.all(), start=(dc == 0), stop=(dc == NDC - 1))
        P.act(rs.all(), stat_bank.all(), AF.Sqrt, scale=1.0 / D, bias=epsb.all())
        P.recip(rs.all(), rs.all())

    def ffn_enqueue(l, which):
        wg, wu, wd = prm["ffn%d_w_gate" % which], prm["ffn%d_w_up" % which], prm["ffn%d_w_down" % which]
        base = len(wq)
        for fcp in range(NFC // 2):
            wq.append(("A", wsrc(wg, l, fcp * 256, 256), 256))
            wq.append(("A", wsrc(wu, l, fcp * 256, 256), 256))
        for dcg in range(4):
            for q in range(4):
                src = wd[l, q * NQF * 128:(q + 1) * NQF * 128, dcg * 512:(dcg + 1) * 512].m(
                    lambda a: a.rearrange("(c p) f -> p c f", p=128))
                wq.append(("D", src, 512))
        return base

    def post_residual(l, gpost, scale, tq):
        rms_rstd(lambda dc: yb[:, dc, :], pb[6], rstd, sq)
        for dc in range(NDC):
            P.stt(yb[:, dc, :], yb[:, dc, :], gpost[:, l * NDC + dc:l * NDC + dc + 1], rstd.all(), ALU.mult, ALU.mult)
            P.stt(xs[:, dc, :], yb[:, dc, :], scale, xs[:, dc, :], ALU.mult, ALU.add)
        P.dma("sp", xt_tile(tq), xs.all())

    def ffn_tile(l, which, tq, base):
        gpre = gains["ffn%d_norm_pre" % which]
        gpost = gains["ffn%d_norm_post" % which]
        P.dma("sp", xs.all(), xt_tile(tq))
        rms_rstd(lambda dc: xs[:, dc, :], pb[6], rstd, sq)
        for dc in range(NDC):
            P.stt(hb[:, dc, :], xs[:, dc, :], gpre[:, l * NDC + dc:l * NDC + dc + 1], rstd.all(), ALU.mult, ALU.mult)
        for fcp in range(NFC // 2):
            wgs = w_get(base + 2 * fcp, depth=3)
            wus = w_get(base + 2 * fcp + 1, depth=2)
            for sub in range(2):
                fc = 2 * fcp + sub
                cs_ = slice(sub * 128, (sub + 1) * 128)
                gps = pb[fc % 2]
                ups = pb[2 + fc % 2]
                for dc in range(NDC):
                    P.mm(gps.all(), wgs[:, dc, cs_], hb[:, dc, :], start=(dc == 0), stop=(dc == NDC - 1))
                for dc in range(NDC):
                    P.mm(ups.all(), wus[:, dc, cs_], hb[:, dc, :], start=(dc == 0), stop=(dc == NDC - 1))
                s_ = sg[fc % 2]
                P.act(s_.all(), gps.all(), AF.Silu)
                P.tt(actb[:, fc, :], s_.all(), ups.all(), ALU.mult)
            w_release(base + 2 * fcp)
            w_release(base + 2 * fcp + 1)
        for dcg in range(4):
            banks = pb[0:4] if dcg % 2 == 0 else pb[4:8]
            for q in range(4):
                kq = base + NFC + dcg * 4 + q
                wds = w_get(kq, depth=2)
                for dj in range(4):
                    for f_ in range(NQF):
                        P.mm(banks[dj].all(), wds[:, f_, dj * 128:(dj + 1) * 128], actb[:, q * NQF + f_, :],
                             start=(q == 0 and f_ == 0), stop=(q == 3 and f_ == NQF - 1))
                w_release(kq)
            for dj in range(4):
                if dj % 2 == 0:
                    P.act(yb[:, dcg * 4 + dj, :], banks[dj].all(), AF.Copy)
                else:
                    P.copy(yb[:, dcg * 4 + dj, :], banks[dj].all())
        post_residual(l, gpost, 0.5, tq)

    def mix_enqueue(l):
        base = len(wq)
        for (kind, c0, wdt, idx) in ZCH:
            wq.append(("A", wsrc(prm["w_in"], l, c0, wdt), wdt))
        for tq in range(NTT):
            for dc in range(NDC):
                wq.append(("A", wsrc(prm["w_out"], l, dc * 128, 128), 128))
        return base

    def mixer(l, base):
        pv0 = l * NPV
        P.release(M0)
        hall = P.sb("hall", [128, NDC, S], BF16)
        mA = P.mark()
        mxs = P.sb("mxs", [128, NDC, TT], F32)
        msq = [P.sb("msq%d" % i, [128, TT], F32) for i in range(2)]
        mrs = P.sb("mrs", [128, TT], F32)
        gpre = gains["mix_norm_pre"]
        for tq in range(NTT):
            P.dma("sp", mxs.all(), xt_tile(tq))
            rms_rstd(lambda dc: mxs[:, dc, :], pb[6], mrs, msq)
            for dc in range(NDC):
                P.stt(hall[:, dc, tq * TT:(tq + 1) * TT], mxs[:, dc, :],
                      gpre[:, l * NDC + dc:l * NDC + dc + 1], mrs.all(), ALU.mult, ALU.mult)
        P.release(mA)
        if cfg.get("mix_stop") == "A":
            return
        cosT = P.sb("cosT", [128, S], F32)
        sinT = P.sb("sinT", [128, S], F32)
        protb = P.sb("protb", [128, 128], BF16)
        wv = P.sb("wv", [128, NDC, DATT], BF16)
        zs = [P.sb("zs%d" % i, [128, S + 1], F32) for i in range(2)]
        zo = [P.sb("zo%d" % i, [128, S], F32) for i in range(2)]
        qko = [P.sb("qko%d" % i, [128, S], BF16) for i in range(2)]
        zb = [P.sb("zb%d" % i, [128, TT], BF16) for i in range(2)]
        t1 = [P.sb("t1%d" % i, [128, TT], F32) for i in range(2)]
        t2 = [P.sb("t2%d" % i, [128, TT], F32) for i in range(2)]
        vst = [P.sb("vst%d" % i, [128, DATT], BF16) for i in range(2)]
        P.dma("sp", cosT.all(), prm["c_cos"].all())
        P.dma("sp", sinT.all(), prm["c_sin"].all())
        P.dma("pool", protb.all(), prm["c_prot"].all())
        P.dma("pool", wv.all(), wsrc(prm["w_in"], l, 1536, DATT))
        for i in range(2):
            P.memset(zs[i][:, 0:1], 0.0)
        for tb in range(S // 128):
            pa = pb[4 + tb % 2]
            pc = pb[6 + tb % 2]
            for dc in range(NDC):
                P.mm(pa.all(), hall[:, dc, tb * 128:(tb + 1) * 128], wv[:, dc, 0:512],
                     start=(dc == 0), stop=(dc == NDC - 1))
            for dc in range(NDC):
                P.mm(pc[:, 0:256], hall[:, dc, tb * 128:(tb + 1) * 128], wv[:, dc, 512:768],
                     start=(dc == 0), stop=(dc == NDC - 1))
            v_ = vst[tb % 2]
            P.act(v_[:, 0:512], pa.all(), AF.Copy)
            P.copy(v_[:, 512:768], pc[:, 0:256])
            P.dma("sp", VT[tb * 128:(tb + 1) * 128, :], v_.all())
        nq = 0
        nz = 0
        if cfg.get("mix_stop") == "V":
            return
        for ci, (kind, c0, wdt, idx) in enumerate(ZCH):
            if cfg.get("mix_stop") == "B%d" % ci:
                return
            wsl = w_get(base + ci)
            if kind in ("q", "k"):
                ob = qko[nq % 2]
            else:
                ob = zo[nz % 2]
                zsb = zs[nz % 2]
            for tq in range(NTT):
                zp = pb[(ci * NTT + tq) % 2]
                tsl = slice(tq * TT, (tq + 1) * TT)
                for dc in range(NDC):
                    P.mm(zp[0:wdt, :], wsl[:, dc, 0:wdt], hall[:, dc, tsl], start=(dc == 0), stop=(dc == NDC - 1))
                if kind in ("q", "k"):
                    k2 = (ci * NTT + tq) % 2
                    P.act(zb[k2].all(), zp.all(), AF.Copy)
                    rp = pb[2 + k2]
                    P.mm(rp.all(), protb.all(), zb[k2].all())
                    P.tt(t1[k2].all(), zp.all(), cosT[:, tsl], ALU.mult)
                    P.tt(t2[k2].all(), rp.all(), sinT[:, tsl], ALU.mult)
                    P.tt(ob[:, tsl], t1[k2].all(), t2[k2].all(), ALU.add, eng="pool")
                elif kind == "pool":
                    P.act(ob[0:wdt, tsl], zp[0:wdt, :], AF.Copy)
                else:
                    P.act(zsb[0:wdt, 1 + tq * TT:1 + (tq + 1) * TT], zp[0:wdt, :], AF.Copy)
            w_release(base + ci)
            if kind in ("q", "k"):
                P.dma("sp", QK[(0 if kind == "q" else 6) + idx], ob.all())
                nq += 1
            else:
                if kind != "pool":
                    mucol = {"rkv": idx, "zw": 18, "za": 19, "zg": 20 + idx}[kind]
                    P.tt(ob[0:wdt, :], zsb[0:wdt, 0:S], zsb[0:wdt, 1:S + 1], ALU.subtract)
                    P.stt(ob[0:wdt, :], ob[0:wdt, :], pvec[0:wdt, pv0 + mucol:pv0 + mucol + 1],
                          zsb[0:wdt, 1:S + 1], ALU.mult, ALU.add)
                zi = {"rkv": ZS_RKV + idx, "zw": ZS_ZW, "za": ZS_ZA, "zg": ZS_ZG + idx, "pool": ZS_POOL + idx}[kind]
                P.dma("sp", ZS[zi, 0:wdt, :], ob[0:wdt, :])
                nz += 1
        P.release(M0)
        if "att" in cfg.get("mixers", ("att", "rwkv", "pool")):
            attention(l)
            P.release(M0)
        if "pool" in cfg.get("mixers", ("att", "rwkv", "pool")):
            pool_mixer(l)
            P.release(M0)
        if "rwkv" in cfg.get("mixers", ("att", "rwkv", "pool")):
            rwkv(l)
            P.release(M0)
        P.top = FFN_TOP
        gpost = gains["mix_norm_post"]
        ob = base + len(ZCH)
        for tq in range(NTT):
            P.dma("sp", xs.all(), xt_tile(tq))
            P.dma("sp", hb.all(), YM[:, :, tq * TT:(tq + 1) * TT].m(lambda a: a.rearrange("c p t -> p c t")))
            for dc in range(NDC):
                wsl = w_get(ob + tq * NDC + dc)
                yps = pb[4 + dc % 2]
                for fc in range(NDC):
                    P.mm(yps.all(), wsl[:, fc, 0:128], hb[:, fc, :], start=(fc == 0), stop=(fc == NDC - 1))
                w_release(ob + tq * NDC + dc)
                P.act(yb[:, dc, :], yps.all(), AF.Copy)
            post_residual(l, gpost, 1.0, tq)

    def attention(l):
        masks = P.sb("amask", [128, 4, 128], BF16)
        P.dma("pool", masks.all(), prm["c_masks"].all())
        qT = P.sb("qT", [128, S], BF16)
        kT = P.sb("kT", [128, S], BF16)
        V1 = P.sb("V1", [128, 16, 128], BF16)
        V4 = P.sb("V4", [128, 4, 4, 128], BF16)
        V16 = P.sb("V16", [128, 16, 128], BF16)
        NUM = P.sb("NUM", [128, S], F32)
        DEN = P.sb("DEN", [128, S], F32)
        yst = P.sb("yst", [128, S], BF16)
        NP_ = 8
        pt = [P.sb("pt%d" % i, [128, 128], BF16) for i in range(NP_)]
        cnt = {"s": 0, "u": 0}
        for j in range(6):
            P.dma("sp", qT.all(), QK[j])
            P.dma("sp", kT.all(), QK[6 + j])
            cs_ = slice(j * 128, (j + 1) * 128)
            P.dma("sp", V1.all(), VT[:, cs_].m(lambda a: a.rearrange("(kb p) c -> p kb c", p=128)))
            for r in range(4):
                P.dma("sp", V4[:, r, :, :], VT[:, cs_].m(
                    lambda a: a.rearrange("(kb p r) c -> p r kb c", p=128, r=4)[:, r]))
            P.dma("sp", V16.all(), VT[:, cs_].m(lambda a: a.rearrange("(p r) c -> p r c", r=16)))
            units = []
            for (d, vsel) in ((1, lambda r, kb, h: V1[:, kb, h * 64:(h + 1) * 64]),
                              (4, lambda r, kb, h: V4[:, r, kb, h * 64:(h + 1) * 64]),
                              (16, lambda r, kb, h: V16[:, r, h * 64:(h + 1) * 64])):
                nb = S // d // 128
                for r in range(d):
                    for qb in range(nb):
                        units.append((d, r, qb, vsel))
            pend = None

            def toks(d, r, b):
                b0 = d * 128 * b + r
                return slice(b0, b0 + d * 127 + 1, d)

            def stage_s(u):
                d, r, qb, vsel = u
                tl = []
                for h in range(2):
                    hp = slice(h * 64, (h + 1) * 64)
                    lst = [(qb, 2)] + ([(qb - 1, 3)] if qb > 0 else [])
                    for (kb, mi) in lst:
                        k_ = cnt["s"] % NP_
                        cnt["s"] += 1
                        sp_ = pb[k_ // 4][:, (k_ % 4) * 128:(k_ % 4 + 1) * 128]
                        P.mm(sp_, kT[hp, toks(d, r, kb)], qT[hp, toks(d, r, qb)])
                        P.act(pt[k_].all(), sp_, AF.Exp, scale=0.125)
                        P.tt(pt[k_].all(), pt[k_].all(), masks[:, mi, :], ALU.mult, eng="pool")
                        tl.append((h, kb, k_))
                return tl

            def stage_pv(u, tl):
                d, r, qb, vsel = u
                ub = cnt["u"] % 2
                cnt["u"] += 1
                nps = pb[2 + ub][:, 0:128]
                dps = pb[4 + ub][:, 0:128]
                nps_h = lambda h: pb[2 + ub][h * 64:(h + 1) * 64, 0:128]
                dps_h = lambda h: pb[4 + ub][h * 64:(h + 1) * 64, 0:128]
                for h in range(2):
                    hp = slice(h * 64, (h + 1) * 64)
                    mine = [t for t in tl if t[0] == h]
                    for i, (_, kb, k_) in enumerate(mine):
                        P.mm(nps_h(h), vsel(r, kb, h), pt[k_].all(),
                             start=(i == 0), stop=(i == len(mine) - 1))
                for h in range(2):
                    hp = slice(h * 64, (h + 1) * 64)
                    mine = [t for t in tl if t[0] == h]
                    for i, (_, kb, k_) in enumerate(mine):
                        P.mm(dps_h(h), onesb[:, 0:64], pt[k_].all(),
                             start=(i == 0), stop=(i == len(mine) - 1))
                tk = toks(d, r, qb)
                if d == 1:
                    P.act(NUM[:, tk], nps, AF.Copy)
                    P.copy(DEN[:, tk], dps)
                else:
                    P.tt(NUM[:, tk], nps, NUM[:, tk], ALU.add)
                    P.tt(DEN[:, tk], dps, DEN[:, tk], ALU.add)

            for u in units:
                tl = stage_s(u)
                if pend is not None:
                    stage_pv(*pend)
                pend = (u, tl)
            stage_pv(*pend)
            P.recip(DEN.all(), DEN.all())
            P.tt(yst.all(), NUM.all(), DEN.all(), ALU.mult)
            P.dma("sp", YM[j], yst.all())

    def pool_mixer(l):
        pv0 = l * NPV
        pw = P.sb("poolw", [128, 4, 128], BF16)
        fac = P.sb("poolfac", [128, 4, 16], F32)
        P.dma("pool", pw.all(), prm["pool_w"][l].m(lambda a: a.rearrange("g c d -> c g d")))
        P.dma("sp", fac.all(), prm["c_poolfac"].all())
        ub = P.sb("pu", [128, 16 + S], F32)
        sa = P.sb("psa", [128, 16 + S], F32)
        sbb = P.sb("psb", [128, 16 + S], F32)
        pbf = P.sb("ppb", [128, S], BF16)
        t16 = P.sb("pt16", [128, 16], F32)
        yo = P.sb("pyo", [128, S], BF16)
        for b_ in (ub, sa, sbb):
            P.memset(b_[:, 0:16], 0.0)
        for g in range(4):
            P.dma("sp", ub[:, 16:16 + S], ZS[ZS_POOL + g])
            cur = ub
            for k in range(g + 1):
                sh = 1 << k
                nxt = sa if cur is not sa else sbb
                P.tt(nxt[:, 16:16 + S], cur[:, 16:16 + S], cur[:, 16 - sh:16 - sh + S], ALU.add)
                cur = nxt
            w = float(1 << (g + 1))
            P.stt(pbf.all(), cur[:, 16:16 + S], 1.0 / w, ub[:, 16:16 + S], ALU.mult, ALU.subtract)
            P.tt(t16.all(), cur[:, 16:32], fac[:, g, :], ALU.mult)
            P.tt(pbf[:, 0:16], t16.all(), ub[:, 16:32], ALU.subtract)
            for tq in range(NTT):
                yp = pb[tq % 2]
                P.mm(yp.all(), pw[:, g, :], pbf[:, tq * TT:(tq + 1) * TT])
                P.act(yo[:, tq * TT:(tq + 1) * TT], yp.all(), AF.Identity,
                      scale=pvec[:, pv0 + 52 + g:pv0 + 52 + g + 1])
            P.dma("sp", YM[12 + g], yo.all())

    def rwkv(l):
        pv0 = l * NPV
        CH = 64
        NCK = TT // CH
        wupb = P.sb("wupb", [96, 768], BF16)
        aupb = P.sb("aupb", [96, 768], BF16)
        gupb = P.sb("gupb", [128, 2, 768], BF16)
        gnw = P.sb("gnw", [128, 6, 64], F32)
        gnb = P.sb("gnb", [128, 6, 64], F32)
        tmask = P.sb("tmask", [128, 4, 128], BF16)
        e2 = P.sb("e2", [128, 64], BF16)
        bones = P.sb("bones", [128, 128], BF16)
        smask = P.sb("smask", [128, TT], F32)
        P.dma("pool", wupb.all(), prm["rwkv_w_up"][l])
        P.dma("pool", aupb.all(), prm["rwkv_a_up"][l])
        P.dma("pool", gupb.all(), prm["rwkv_g_up"][l].m(lambda a: a.rearrange("(c p) f -> p c f", p=128)))
        P.dma("sp", gnw.all(), prm["gnw"][l].m(lambda a: a.rearrange("p (j v) -> p j v", j=6)))
        P.dma("sp", gnb.all(), prm["gnb"][l].m(lambda a: a.rearrange("p (j v) -> p j v", j=6)))
        P.dma("pool", tmask.all(), prm["c_masks"].all())
        P.dma("pool", e2.all(), prm["c_e2"].all())
        P.dma("pool", bones.all(), prm["c_blockones"].all())
        P.dma("sp", smask.all(), prm["c_scanmask"].all())
        SU, SL, IU = 0, 1, 2
        NG = 3
        rin = P.sb("rin", [128, 9, TT], F32)
        zwf = P.sb("zwf", [96, TT], F32)
        zaf = P.sb("zaf", [96, TT], F32)
        zgf = P.sb("zgf", [128, 2, TT], F32)
        twb = P.sb("twb", [96, TT], BF16)
        zab = P.sb("zab", [96, TT], BF16)
        sgz = P.sb("sgz", [128, 2, TT], BF16)
        f = {nm: P.sb("f_" + nm, [128, TT], F32) for nm in
             ["sig", "a", "cs", "cse", "ei", "en", "ee", "kk", "kkn", "t", "km", "bv"]}
        kk2 = P.sb("kk2", [128, TT], BF16)
        bd = {nm: [P.sb("bd_%s%d" % (nm, jj), [128, NCK, 128], BF16) for jj in range(NG)]
              for nm in ["R", "A", "K", "B", "V", "X"]}
        wc = [P.sb("wc%d" % jj, [128, NCK], F32) for jj in range(NG)]
        gst = P.sb("gst", [128, NCK, NG, 64], F32)
        yoT = P.sb("yoT", [128, NG, S], BF16)
        Mf = P.sb("Mf", [128, NG, 64], F32)
        Mb = P.sb("Mb", [128, NG, 64], BF16)
        NSL = 2
        S4 = [P.sb("S4_%d" % k, [128, NG, 4, 128], BF16) for k in range(NSL)]
        RKT = [P.sb("RKT_%d" % k, [128, NG, 128], BF16) for k in range(NSL)]
        KB = [P.sb("KB_%d" % k, [128, NG, 2, 128], BF16) for k in range(NSL)]
        VST = [P.sb("VST_%d" % k, [128, NG, 64], BF16) for k in range(NSL)]
        BON = [P.sb("BON_%d" % k, [128, NG, 1], F32) for k in range(NSL)]
        QQ = [[P.sb("QQ_%d_%d" % (k, i), [128, NG, 128], BF16) for i in range(2)] for k in range(NSL)]
        PP = [[P.sb("PP_%d_%d" % (k, i), [128, NG, 2, 128], BF16) for i in range(2)] for k in range(NSL)]
        M4 = P.sb("M4", [128, NG, 4, 128], BF16)
        MIU3 = P.sb("MIU3", [128, NG, 128], BF16)
        I3 = P.sb("I3", [128, NG, 128], BF16)
        for jj in range(NG):
            for i_, mi in enumerate((SU, SL, SU, IU)):
                P.copy(M4[:, jj, i_, :], tmask[:, mi, :])
            P.copy(MIU3[:, jj, :], tmask[:, IU, :])
            P.copy(I3[:, jj, :], identb.all())
        Xb = P.sb("Xb", [128, NG, 64], BF16)
        Ub = P.sb("Ub", [128, NG, 64], BF16)
        r2tmp = P.sb("r2tmp", [128, NG, 64], F32)
        r2yn = P.sb("r2yn", [128, NG, 64], F32)
        r2st = P.sb("r2st", [128, NG, 6], F32)
        r2mv = P.sb("r2mv", [128, NG, 2], F32)
        r2rs = P.sb("r2rs", [128, NG], F32)
        YO = P.sb("YO", [128, NG, 128], BF16)
        f3 = lambda vw, n: vw.m(lambda a: a.rearrange("p (j v) -> p j v", j=NG))
        for grp in range(2):
            pairs = [grp * NG + jj for jj in range(NG)]
            for nm in bd:
                for jj in range(NG):
                    P.memset(bd[nm][jj].all(), 0.0, eng="pool")
            P.memset(YO.all(), 0.0)
            P.memset(Mf.all(), 0.0)
            P.memset(Mb.all(), 0.0)
            for tq in range(NTT):
                tsl = slice(tq * TT, (tq + 1) * TT)
                for q3 in range(3):
                    P.dma("sp", rin[:, q3 * 3:(q3 + 1) * 3, :],
                          ZS[ZS_RKV + q3 * 6 + grp * NG:ZS_RKV + q3 * 6 + grp * NG + NG, :, tsl].m(
                              lambda a: a.rearrange("c p t -> p c t")))
                P.dma("sp", zwf.all(), ZS[ZS_ZW, 0:96, tsl])
                P.dma("sp", zaf.all(), ZS[ZS_ZA, 0:96, tsl])
                P.dma("sp", zgf.all(), ZS[ZS_ZG:ZS_ZG + 2, :, tsl].m(lambda a: a.rearrange("c p t -> p c t")))
                P.act(twb.all(), zwf.all(), AF.Tanh)
                P.act(zab.all(), zaf.all(), AF.Copy)
                P.act(sgz.all(), zgf.all(), AF.Sigmoid)
                for jj, j in enumerate(pairs):
                    r_ = rin[:, jj, :]
                    k_ = rin[:, 3 + jj, :]
                    v_ = rin[:, 6 + jj, :]
                    pcol = lambda o: pvec[:, pv0 + o + j:pv0 + o + j + 1]
                    xw = pb[0]
                    P.mm(xw.all(), wupb[:, j * 128:(j + 1) * 128], twb.all())
                    P.act(f["sig"].all(), xw.all(), AF.Sigmoid, bias=pcol(40))
                    ap_ = pb[1]
                    P.mm(ap_.all(), aupb[:, j * 128:(j + 1) * 128], zab.all())
                    P.act(f["a"].all(), ap_.all(), AF.Sigmoid, bias=pcol(34))
                    P.scan(f["cs"].all(), smask.all(), f["sig"].all(), 0.0, ALU.mult, ALU.add)
                    P.tt(f["cse"].all(), f["cs"].all(), f["sig"].all(), ALU.subtract, eng="pool")
                    P.act(f["ei"].all(), f["cs"].all(), AF.Exp, scale=-C05)
                    P.act(f["en"].all(), f["cs"].all(), AF.Exp, scale=C05)
                    P.act(f["ee"].all(), f["cse"].all(), AF.Exp, scale=-C05)
                    P.ts(f["kk"].all(), k_, pcol(22), None, ALU.mult)
                    P.tt(kk2.all(), f["kk"].all(), f["kk"].all(), ALU.mult, eng="pool")
                    ssp = pb[2]
                    P.mm(ssp.all(), bones.all(), kk2.all())
                    P.act(f["kkn"].all(), ssp.all(), AF.Sqrt)
                    P.ts(f["kkn"].all(), f["kkn"].all(), 1e-12, None, ALU.max)
                    P.recip(f["kkn"].all(), f["kkn"].all())
                    P.tt(f["kkn"].all(), f["kkn"].all(), f["kk"].all(), ALU.mult)
                    P.ts(f["t"].all(), f["a"].all(), pcol(28), omka[:, l * 6 + j:l * 6 + j + 1], ALU.mult, ALU.add)
                    P.tt(f["km"].all(), k_, f["t"].all(), ALU.mult, eng="pool")
                    P.tt(f["bv"].all(), f["kkn"].all(), f["a"].all(), ALU.mult, eng="pool")
                    for h in range(2):
                        hp = slice(h * 64, (h + 1) * 64)
                        v3 = lambda vw: vw.m(lambda a: a.rearrange("p (c t) -> p c t", t=CH))
                        dst = lambda nm: bd[nm][jj][hp, :, h * 64:(h + 1) * 64]
                        P.tt(dst("R"), v3(rin[hp, jj, :]), v3(f["ei"][hp, :]), ALU.mult)
                        P.stt(dst("A"), v3(f["kkn"][hp, :]), -1.0, v3(f["ee"][hp, :]), ALU.mult, ALU.mult)
                        P.tt(dst("K"), v3(f["km"][hp, :]), v3(f["en"][hp, :]), ALU.mult, eng="pool")
                        P.tt(dst("B"), v3(f["bv"][hp, :]), v3(f["en"][hp, :]), ALU.mult, eng="pool")
                        P.act(dst("V"), v3(rin[hp, 6 + jj, :]), AF.Copy)
                        P.stt(dst("X"), v3(rin[hp, jj, :]), pvec[hp, pv0 + 46 + j:pv0 + 46 + j + 1],
                              v3(f["km"][hp, :]), ALU.mult, ALU.mult)
                    P.copy(wc[jj].all(), f["ei"][:, CH - 1:TT:CH])
                for c8 in range(NCK):
                    gp = pb[3]
                    for h in range(2):
                        for kc in range(2):
                            hd0 = (2 * pairs[0] + h) * 64
                            rhs = View(gupb.t[:, kc, :].rearrange("p (j x) -> p j x", x=128)[
                                       :, pairs[0]:pairs[0] + NG, h * 64:(h + 1) * 64], gupb,
                                       ((0, 128), (kc, kc + 1), (0, 768)))
                            P.mm(gp[h * 64:(h + 1) * 64, 0:NG * 64],
                                 sgz[:, kc, c8 * CH:(c8 + 1) * CH], rhs, start=(kc == 0), stop=(kc == 1))
                    P.act(gst[:, c8, :, :], gp[:, 0:NG * 64].m(lambda a: a.rearrange("p (j v) -> p j v", v=64)),
                          AF.Copy)

                def R1(c8):
                    k = c8 % NSL
                    B_ = lambda nm, jj: bd[nm][jj][:, c8, :]
                    reg = lambda jj, i, a=0, z=128: pb[2 * jj + i // 4][:, (i % 4) * 128 + a:(i % 4) * 128 + z]
                    for jj in range(NG):
                        P.mm(reg(jj, 0), B_("B", jj), B_("A", jj))
                        P.mm(reg(jj, 1), B_("A", jj), B_("B", jj))
                        P.mm(reg(jj, 2), B_("K", jj), B_("A", jj))
                        P.mm(reg(jj, 3), B_("B", jj), B_("R", jj))
                        P.mm(reg(jj, 4), B_("K", jj), B_("R", jj))
                        P.mm(reg(jj, 5), B_("K", jj), identb.all())
                        P.mm(reg(jj, 6), B_("B", jj), identb.all())
                        P.mm(reg(jj, 7, 0, 64), B_("V", jj), e2.all())
                        P.mm(reg(jj, 7, 64, 65), B_("X", jj), onesb[:, 0:1])
                    yield
                    fl = lambda vw: vw.m(lambda a: a.rearrange("p j r c -> p j (r c)"))
                    P.tt(fl(S4[k].all()), PSF[:, 0:6:2, :], fl(M4.all()), ALU.mult)
                    P.tt(RKT[k].all(), PSF[:, 1:7:2, 0:128], MIU3.all(), ALU.mult)
                    P.act(fl(KB[k].all()), PSF[:, 1:7:2, 128:384], AF.Copy)
                    P.act(VST[k].all(), PSF[:, 1:7:2, 384:448], AF.Copy)
                    P.act(BON[k].all(), PSF[:, 1:7:2, 448:449], AF.Copy)
                    P.tt(QQ[k][0].all(), S4[k][:, :, 0, :], I3.all(), ALU.add)
                    yield
                    for lev in range(1, 7):
                        if lev == 1:
                            Pc = lambda jj: S4[k][:, jj, 0, :]
                            PTc = lambda jj: S4[k][:, jj, 1, :]
                        else:
                            Pc = (lambda pp: (lambda jj: pp[:, jj, 0, :]))(PP[k][lev % 2])
                            PTc = (lambda pp: (lambda jj: pp[:, jj, 1, :]))(PP[k][lev % 2])
                        PPn = PP[k][(lev + 1) % 2]
                        Qc, Qn = QQ[k][lev % 2], QQ[k][(lev + 1) % 2]
                        for jj in range(NG):
                            if lev <= 4:
                                P.mm(reg(jj, 0), PTc(jj), Pc(jj))
                            if lev <= 5:
                                P.mm(reg(jj, 1), Pc(jj), PTc(jj))
                            if lev >= 2:
                                P.mm(reg(jj, 4), PTc(jj), Qc[:, jj, :])
                        yield
                        if lev <= 4:
                            P.act(PPn.all().m(lambda a: a.rearrange("p j r c -> p j (r c)")),
                                  PSF[:, 0:6:2, 0:256], AF.Copy)
                        elif lev == 5:
                            P.act(PPn[:, :, 1, :], PSF[:, 0:6:2, 128:256], AF.Copy)
                        if lev >= 2:
                            P.tt(Qn.all(), PSF[:, 1:7:2, 0:128], Qc.all(), ALU.add)
                        yield

                def R2(c8):
                    k = c8 % NSL
                    B_ = lambda nm, jj: bd[nm][jj][:, c8, :]
                    xr = lambda jj: pb[6][:, jj * 64:(jj + 1) * 64]
                    ur = lambda jj: pb[6][:, 192 + jj * 64:192 + (jj + 1) * 64]
                    yr = lambda jj: pb[7][:, jj * 64:(jj + 1) * 64]
                    dr = lambda jj: pb[7][:, 192 + jj * 64:192 + (jj + 1) * 64]
                    for jj in range(NG):
                        P.mm(xr(jj), B_("A", jj), Mb[:, jj, :], start=True, stop=False)
                        P.mm(xr(jj), S4[k][:, jj, 2, :], VST[k][:, jj, :], start=False, stop=True)
                    yield
                    P.act(Xb.all(), f3(pb[6][:, 0:192], NG), AF.Copy)
                    yield
                    for jj in range(NG):
                        P.mm(ur(jj), QQ[k][1][:, jj, :], Xb[:, jj, :])
                    yield
                    P.act(Ub.all(), f3(pb[6][:, 192:384], NG), AF.Copy)
                    yield
                    for jj in range(NG):
                        P.mm(yr(jj), B_("R", jj), Mb[:, jj, :], start=True, stop=False)
                        P.mm(yr(jj), S4[k][:, jj, 3, :], Ub[:, jj, :], start=False, stop=False)
                        P.mm(yr(jj), RKT[k][:, jj, :], VST[k][:, jj, :], start=False, stop=True)
                    for jj in range(NG):
                        P.mm(dr(jj), KB[k][:, jj, 1, :], Ub[:, jj, :], start=True, stop=False)
                        P.mm(dr(jj), KB[k][:, jj, 0, :], VST[k][:, jj, :], start=False, stop=True)
                    yield
                    P.tt(r2tmp.all(), f3(pb[7][:, 192:384], NG), Mf.all(), ALU.add)
                    for jj in range(NG):
                        P.ts(Mf[:, jj, :], r2tmp[:, jj, :], wc[jj][:, c8:c8 + 1], None, ALU.mult)
                    P.act(Mb.all(), Mf.all(), AF.Copy)
                    for jj in range(NG):
                        P.bn_stats(r2st[:, jj, :], yr(jj))
                        P.bn_aggr(r2mv[:, jj, :], r2st[:, jj, :])
                    P.act(r2rs.all(), r2mv[:, :, 1], AF.Sqrt, bias=gneps.all())
                    P.recip(r2rs.all(), r2rs.all())
                    for jj in range(NG):
                        P.ts(r2yn[:, jj, :], yr(jj), r2mv[:, jj, 0:1], r2rs[:, jj:jj + 1], ALU.subtract, ALU.mult)
                    P.tt(r2yn.all(), r2yn.all(), gnw[:, grp * NG:(grp + 1) * NG, :], ALU.mult)
                    P.tt(r2yn.all(), r2yn.all(), gnb[:, grp * NG:(grp + 1) * NG, :], ALU.add)
                    for jj in range(NG):
                        P.stt(r2yn[:, jj, :], VST[k][:, jj, :], BON[k][:, jj, :], r2yn[:, jj, :], ALU.mult, ALU.add)
                    for h in range(2):
                        hp = slice(h * 64, (h + 1) * 64)
                        P.tt(YO[hp, :, h * 64:(h + 1) * 64], r2yn[hp, :, :], gst[hp, c8, :, :], ALU.mult)
                    yield
                    for jj in range(NG):
                        P.mm(xr(jj), YO[:, jj, :], e2.all())
                    yield
                    P.act(yoT[:, :, tq * TT + c8 * CH:tq * TT + (c8 + 1) * CH], f3(pb[6][:, 0:192], NG), AF.Copy)

                for step in range(NCK + 1):
                    gens = []
                    if step < NCK:
                        gens.append(R1(step))
                    if step > 0:
                        gens.append(R2(step - 1))
                    rr(gens)
            for jj, j in enumerate(pairs):
                P.dma("sp", YM[6 + j], yoT[:, jj, :])

    input_transpose()
    phases = cfg.get("phases", ("ffn1", "mix", "ffn2"))
    plan = []
    for l in range(n_layers):
        for ph in phases:
            plan.append((l, ph))
    bases = {}
    for (l, ph) in plan:
        if ph in ("ffn1", "ffn2"):
            which = 1 if ph == "ffn1" else 2
            for tq in range(NTT):
                bases[(l, ph, tq)] = ffn_enqueue(l, which)
        else:
            bases[(l, ph)] = mix_enqueue(l)
    for (l, ph) in plan:
        if ph in ("ffn1", "ffn2"):
            which = 1 if ph == "ffn1" else 2
            P.top = FFN_TOP
            for tq in range(NTT):
                ffn_tile(l, which, tq, bases[(l, ph, tq)])
        else:
            mixer(l, bases[(l, ph)])
    P.top = FFN_TOP
    output_transpose()
    for k in list(wstate["slots"]):
        P.s.final.append(("pool", wstate["dix"][k]))
    P.s.emit(nc, es)
    es.close()
    return nc


def host_consts():
    c = {}
    c["c_ident"] = np.eye(128, dtype=np.float32)
    prot = np.zeros((128, 128), np.float32)
    for m in range(128):
        if m % 64 < 32:
            prot[m + 32, m] = -1.0
        else:
            prot[m - 32, m] = 1.0
    c["c_prot"] = prot
    inv = (10000.0 ** (-np.arange(0, 64, 2, dtype=np.float32) / 64)).astype(np.float32)
    ang = np.arange(S, dtype=np.float32)[:, None] * inv[None, :]
    cs, sn = np.cos(ang).astype(np.float32), np.sin(ang).astype(np.float32)
    rows = np.arange(128) % 32
    c["c_cos"] = np.ascontiguousarray(cs.T[rows])
    c["c_sin"] = np.ascontiguousarray(sn.T[rows])
    p = np.arange(128)[:, None]
    f_ = np.arange(128)[None, :]
    c["c_masks"] = np.ascontiguousarray(
        np.stack([(p < f_), (p > f_), (p <= f_), (p >= f_)], axis=1).astype(np.float32))
    c["c_e2"] = np.concatenate([np.eye(64, dtype=np.float32)] * 2, axis=0)
    sm = np.ones((128, 512), np.float32)
    sm[:, ::64] = 0.0
    c["c_scanmask"] = sm
    pf = np.zeros((128, 4, 16), np.float32)
    for g in range(4):
        w = 2 ** (g + 1)
        pf[:, g, :] = 1.0 / np.minimum(np.arange(1, 17), w)
    c["c_poolfac"] = pf
    c["c_blockones"] = ((p // 64) == (f_ // 64)).astype(np.float32)
    return c


def make_in_map(inputs, b, consts=None):
    m = {"x": np.ascontiguousarray(inputs["x"][b])}
    for nm in ["ffn1_norm_pre", "ffn1_norm_post", "mix_norm_pre", "mix_norm_post",
               "ffn2_norm_pre", "ffn2_norm_post"]:
        m[nm] = np.ascontiguousarray(inputs[nm]).reshape(L * NDC, 128)
    for nm in ["ffn1_w_gate", "ffn1_w_up", "ffn1_w_down", "w_in", "w_out",
               "ffn2_w_gate", "ffn2_w_up", "ffn2_w_down", "rwkv_w_up", "rwkv_a_up", "rwkv_g_up", "pool_w"]:
        m[nm] = np.ascontiguousarray(inputs[nm])
    pv = np.zeros((L, NPV, 128), np.float32)
    mu = inputs["rwkv_mu"]
    pv[:, 0:18, :] = mu[:, 0:2304].reshape(L, 18, 128)
    pv[:, 18, 0:96] = mu[:, 2304:2400]
    pv[:, 19, 0:96] = mu[:, 2400:2496]
    pv[:, 20:22, :] = mu[:, 2496:2752].reshape(L, 2, 128)
    pv[:, 22:28, :] = inputs["rwkv_k_k"].reshape(L, 6, 128)
    pv[:, 28:34, :] = inputs["rwkv_k_a"].reshape(L, 6, 128)
    pv[:, 34:40, :] = inputs["rwkv_a0"].reshape(L, 6, 128)
    pv[:, 40:46, :] = inputs["rwkv_w0"].reshape(L, 6, 128)
    pv[:, 46:52, :] = inputs["rwkv_r_k"].reshape(L, 6, 128)
    pv[:, 52:56, :] = inputs["pool_scale"].reshape(L, 4, 128)
    m["pvec"] = pv.reshape(L * NPV, 128)
    for nm, key in (("gnw", "rwkv_gn_w"), ("gnb", "rwkv_gn_b")):
        g = inputs[key].reshape(L, 6, 2, 64)
        g = np.transpose(g, (0, 2, 1, 3))
        m[nm] = np.ascontiguousarray(np.repeat(g, 64, axis=1).reshape(L, 128, 384))
    m.update(consts if consts is not None else host_consts())
    return m


def kernel(**inputs):
    inputs = {k: np.asarray(v, dtype=np.float32) for k, v in inputs.items()}
    nc = build({})
    consts = host_consts()
    in_maps = [make_in_map(inputs, b, consts) for b in range(8)]
    res = run_bass_kernel_spmd(nc, in_maps, core_ids=list(range(8)))
    return np.stack([r["out"] for r in res.results], axis=0).astype(np.float32)
```
